# Optimizing a Trainium2 kernel written in Bass

```python
import jax, jax.numpy as jnp
from jax import lax
import numpy as np

D_MODEL = 1024
BATCH = 2
SEQ = 8192
DEPTH = 2

D_MIX = D_MODEL
RW_HEADS = 8
RW_HEAD_DIM = 64
RW_WIDTH = RW_HEADS * RW_HEAD_DIM
DECAY_LORA = 64
AAA_LORA = 64
GATE_LORA = 128
N_DIR = 2
MLA_HEADS = 8
QK_NOPE = 64
QK_ROPE = 32
V_HEAD = 64
Q_LORA = 256
KV_LORA = 128
MLA_WIDTH = MLA_HEADS * V_HEAD
ROPE_THETA = 10000.0
Q_BLOCK = 128
N_EXPERTS = 16
EC_FACTOR = 2
D_EXPERT = 1024
D_PLE = 256
NORM_EPS = 1e-6
GN_EPS = 64e-5

RW_COLS = 3 * RW_WIDTH + N_DIR * DECAY_LORA + N_DIR * AAA_LORA + GATE_LORA
MLA_COLS = Q_LORA + KV_LORA + QK_ROPE
IN_COLS = RW_COLS + MLA_COLS

kernel_name = 'hybrid_rwkv7_mla_expert_choice_encoder'


def rmsnorm(x, g, eps=NORM_EPS):
    xf = x.astype(jnp.float32)
    y = xf * lax.rsqrt(jnp.mean(xf * xf, axis=-1, keepdims=True) + eps)
    return (y * g.astype(jnp.float32)).astype(x.dtype)


def centred_shift_mix(z, mu):
    zp = jnp.pad(z, ((0, 0), (1, 1), (0, 0)))
    nb = 0.5 * (zp[:, :-2] + zp[:, 2:])
    return z + mu * (nb - z)


def rwkv7_scan(r, w, k, v, kk, a, reverse):
    b, _, h, n = r.shape
    xs = tuple(jnp.moveaxis(t, 1, 0) for t in (r, w, k, v, kk, a))
    s0 = jnp.zeros((b, h, n, n), jnp.float32)

    def step(s, inp):
        r_t, w_t, k_t, v_t, kk_t, a_t = inp
        sa = jnp.einsum('bhvk,bhk->bhv', s, -kk_t)
        s = (s * w_t[:, :, None, :] + sa[..., None] * (kk_t * a_t)[:, :, None, :]
             + v_t[..., None] * k_t[:, :, None, :])
        return s, jnp.einsum('bhvk,bhk->bhv', s, r_t)

    _, out = lax.scan(step, s0, xs, reverse=reverse)
    return jnp.moveaxis(out, 0, 1)


def rwkv7_group(z, mu, w0, w_up, a0, a_up, g_up, k_k, k_a, r_k, lnx_w, lnx_b):
    z = centred_shift_mix(z, mu).astype(jnp.float32)
    b, t, _ = z.shape
    o1 = RW_WIDTH
    o2 = 2 * RW_WIDTH
    o3 = 3 * RW_WIDTH
    o4 = o3 + N_DIR * DECAY_LORA
    o5 = o4 + N_DIR * AAA_LORA
    r = z[..., :o1]
    k = z[..., o1:o2]
    v = z[..., o2:o3]
    wd = z[..., o3:o4].reshape(b, t, N_DIR, DECAY_LORA)
    ad = z[..., o4:o5].reshape(b, t, N_DIR, AAA_LORA)
    gd = z[..., o5:]
    w_logit = w0 + jnp.einsum('btdl,dlc->btdc', jnp.tanh(wd), w_up)
    decay = jnp.exp(-jnp.exp(-jax.nn.softplus(-w_logit) - 0.5))
    a = jax.nn.sigmoid(a0 + jnp.einsum('btdl,dlc->btdc', ad, a_up))
    g = jax.nn.sigmoid(gd) @ g_up

    def hs(u):
        return u.reshape(u.shape[:-1] + (RW_HEADS, RW_HEAD_DIM))

    kk = hs(k * k_k)
    kk = kk / jnp.maximum(jnp.sqrt(jnp.sum(kk * kk, axis=-1, keepdims=True)), 1e-12)
    k_dir = k[:, :, None, :] * (1.0 + (a - 1.0) * k_a)
    r_h = hs(r)
    v_h = hs(v)
    o_fwd = rwkv7_scan(r_h, hs(decay[:, :, 0]), hs(k_dir[:, :, 0]), v_h, kk, hs(a[:, :, 0]), False)
    o_bwd = rwkv7_scan(r_h, hs(decay[:, :, 1]), hs(k_dir[:, :, 1]), v_h, kk, hs(a[:, :, 1]), True)
    o = o_fwd + o_bwd
    mean = jnp.mean(o, axis=-1, keepdims=True)
    var = jnp.mean(jnp.square(o - mean), axis=-1, keepdims=True)
    o = ((o - mean) * lax.rsqrt(var + GN_EPS)).reshape(b, t, RW_WIDTH) * lnx_w + lnx_b
    k_bonus = hs(0.5 * (k_dir[:, :, 0] + k_dir[:, :, 1]))
    bonus = (jnp.sum(r_h * k_bonus * r_k, axis=-1, keepdims=True) * v_h).reshape(b, t, RW_WIDTH)
    return (o + bonus) * g


def rope_angles(positions):
    inv = ROPE_THETA ** (-jnp.arange(0, QK_ROPE, 2, dtype=jnp.float32) / QK_ROPE)
    ang = positions.astype(jnp.float32)[..., None] * inv
    return jnp.cos(ang), jnp.sin(ang)


def apply_rope(u, cos, sin):
    half = QK_ROPE // 2
    u1 = u[..., :half]
    u2 = u[..., half:]
    return jnp.concatenate([u1 * cos - u2 * sin, u1 * sin + u2 * cos], axis=-1)


def mla_group(z, cos, sin, q_norm, q_up, kv_norm, kv_up, o_norm):
    b, t, _ = z.shape
    qd = z[..., :Q_LORA]
    kvd = z[..., Q_LORA:Q_LORA + KV_LORA]
    kr = z[..., Q_LORA + KV_LORA:]
    q = (rmsnorm(qd, q_norm) @ q_up).reshape(b, t, MLA_HEADS, QK_NOPE + QK_ROPE)
    q_nope = q[..., :QK_NOPE]
    q_rope = apply_rope(q[..., QK_NOPE:], cos[:, :, None, :], sin[:, :, None, :])
    kv = (rmsnorm(kvd, kv_norm) @ kv_up).reshape(b, t, MLA_HEADS, QK_NOPE + V_HEAD)
    k_nope = kv[..., :QK_NOPE]
    v = kv[..., QK_NOPE:]
    k_rope = apply_rope(kr, cos, sin)
    scale = (QK_NOPE + QK_ROPE) ** -0.5
    nblk = t // Q_BLOCK

    def to_blocks(u):
        return jnp.moveaxis(u.reshape((b, nblk, Q_BLOCK) + u.shape[2:]), 1, 0)

    def attend(qs):
        qn, qr = qs
        s = (jnp.einsum('bqhd,bkhd->bhqk', qn, k_nope).astype(jnp.float32)
             + jnp.einsum('bqhr,bkr->bhqk', qr, k_rope).astype(jnp.float32)) * scale
        pr = jax.nn.softmax(s, axis=-1).astype(v.dtype)
        return jnp.einsum('bhqk,bkhd->bqhd', pr, v)

    o = lax.map(attend, (to_blocks(q_nope), to_blocks(q_rope)))
    o = jnp.moveaxis(o, 0, 1).reshape(b, t, MLA_WIDTH)
    return rmsnorm(o, o_norm)


def expert_choice_ffn(h, router, w_gate, w_up, w_down):
    b, t, _ = h.shape
    cap = EC_FACTOR * t // N_EXPERTS
    aff = jax.nn.softmax((h @ router).astype(jnp.float32), axis=-1)
    gate, idx = lax.top_k(jnp.swapaxes(aff, 1, 2), cap)
    b_idx = jnp.arange(b)[:, None, None]
    xe = h[b_idx, idx]
    hid = (jax.nn.silu(jnp.einsum('becd,edf->becf', xe, w_gate))
           * jnp.einsum('becd,edf->becf', xe, w_up))
    ye = jnp.einsum('becf,efd->becd', hid, w_down) * gate[..., None].astype(h.dtype)
    return jnp.zeros_like(h).at[b_idx, idx].add(ye.astype(h.dtype))


def setup_inputs(seed: int = 0) -> dict:
    key = jax.random.key(seed)
    ks = jax.random.split(key, 40)
    f32 = jnp.float32

    def nrm(k, shape, scale):
        return jax.random.normal(k, shape, f32) * scale

    def gain(k, shape):
        return 1.0 + 0.02 * jax.random.normal(k, shape, f32)

    offsets = jax.random.randint(ks[2], (BATCH, 1), 0, 1024, dtype=jnp.int32)
    positions = jnp.arange(SEQ, dtype=jnp.int32)[None, :] + offsets
    return {
        'x': nrm(ks[0], (BATCH, SEQ, D_MODEL), 1.0),
        'p': nrm(ks[1], (DEPTH, BATCH, SEQ, D_PLE), 1.0),
        'positions': positions,
        'attn_norm': gain(ks[3], (DEPTH, D_MODEL)),
        'w_in': nrm(ks[4], (DEPTH, D_MODEL, IN_COLS), D_MODEL ** -0.5),
        'rw_mu': jax.random.uniform(ks[5], (DEPTH, RW_COLS), f32),
        'rw_w0': jax.random.uniform(ks[6], (DEPTH, N_DIR, RW_WIDTH), f32, -5.0, 1.0),
        'rw_w_up': nrm(ks[7], (DEPTH, N_DIR, DECAY_LORA, RW_WIDTH), 0.5 * DECAY_LORA ** -0.5),
        'rw_a0': nrm(ks[8], (DEPTH, N_DIR, RW_WIDTH), 0.5),
        'rw_a_up': nrm(ks[9], (DEPTH, N_DIR, AAA_LORA, RW_WIDTH), AAA_LORA ** -0.5),
        'rw_g_up': nrm(ks[10], (DEPTH, GATE_LORA, RW_WIDTH), GATE_LORA ** -0.5),
        'rw_k_k': 0.85 + 0.1 * jax.random.normal(ks[11], (DEPTH, RW_WIDTH), f32),
        'rw_k_a': 1.0 + 0.1 * jax.random.normal(ks[12], (DEPTH, RW_WIDTH), f32),
        'rw_r_k': nrm(ks[13], (DEPTH, RW_HEADS, RW_HEAD_DIM), 0.1),
        'rw_lnx_w': gain(ks[14], (DEPTH, RW_WIDTH)),
        'rw_lnx_b': nrm(ks[15], (DEPTH, RW_WIDTH), 0.02),
        'mla_q_norm': gain(ks[16], (DEPTH, Q_LORA)),
        'mla_q_up': nrm(ks[17], (DEPTH, Q_LORA, MLA_HEADS * (QK_NOPE + QK_ROPE)), Q_LORA ** -0.5),
        'mla_kv_norm': gain(ks[18], (DEPTH, KV_LORA)),
        'mla_kv_up': nrm(ks[19], (DEPTH, KV_LORA, MLA_HEADS * (QK_NOPE + V_HEAD)), KV_LORA ** -0.5),
        'mla_o_norm': gain(ks[20], (DEPTH, MLA_WIDTH)),
        'w_out': nrm(ks[21], (DEPTH, D_MIX, D_MODEL), D_MIX ** -0.5),
        'ffn_norm': gain(ks[22], (DEPTH, D_MODEL)),
        'router': nrm(ks[23], (DEPTH, D_MODEL, N_EXPERTS), D_MODEL ** -0.5),
        'exp_w_gate': nrm(ks[24], (DEPTH, N_EXPERTS, D_MODEL, D_EXPERT), D_MODEL ** -0.5),
        'exp_w_up': nrm(ks[25], (DEPTH, N_EXPERTS, D_MODEL, D_EXPERT), D_MODEL ** -0.5),
        'exp_w_down': nrm(ks[26], (DEPTH, N_EXPERTS, D_EXPERT, D_MODEL), D_EXPERT ** -0.5),
        'ple_norm': gain(ks[27], (DEPTH, D_MODEL)),
        'ple_proj': nrm(ks[28], (DEPTH, D_PLE, D_MODEL), D_PLE ** -0.5),
        'ple_gate': nrm(ks[29], (DEPTH, D_MODEL, D_MODEL), D_MODEL ** -0.5),
        'final_norm': gain(ks[30], (D_MODEL,)),
    }


def reference(x, p, positions, attn_norm, w_in, rw_mu, rw_w0, rw_w_up, rw_a0, rw_a_up,
              rw_g_up, rw_k_k, rw_k_a, rw_r_k, rw_lnx_w, rw_lnx_b, mla_q_norm, mla_q_up,
              mla_kv_norm, mla_kv_up, mla_o_norm, w_out, ffn_norm, router, exp_w_gate,
              exp_w_up, exp_w_down, ple_norm, ple_proj, ple_gate, final_norm):
    cos, sin = rope_angles(positions)
    for i in range(DEPTH):
        h = rmsnorm(x, attn_norm[i])
        z = h @ w_in[i]
        y_rw = rwkv7_group(z[..., :RW_COLS], rw_mu[i], rw_w0[i], rw_w_up[i], rw_a0[i],
                           rw_a_up[i], rw_g_up[i], rw_k_k[i], rw_k_a[i], rw_r_k[i],
                           rw_lnx_w[i], rw_lnx_b[i])
        y_mla = mla_group(z[..., RW_COLS:], cos, sin, mla_q_norm[i], mla_q_up[i],
                          mla_kv_norm[i], mla_kv_up[i], mla_o_norm[i])
        y = jnp.concatenate([y_rw.astype(x.dtype), y_mla.astype(x.dtype)], axis=-1)
        x = x + (y @ w_out[i]).astype(x.dtype)
        x = x + expert_choice_ffn(rmsnorm(x, ffn_norm[i]), router[i], exp_w_gate[i],
                                  exp_w_up[i], exp_w_down[i])
        ple = (p[i] @ ple_proj[i]) * jax.nn.sigmoid(rmsnorm(x, ple_norm[i]) @ ple_gate[i])
        x = x + ple.astype(x.dtype)
    return rmsnorm(x, final_norm)
```

```python
import numpy as np
from contextlib import ExitStack
import concourse.bass as bass
import concourse.mybir as mybir
from concourse.bass_utils import run_bass_kernel_spmd

F32 = mybir.dt.float32
BF16 = mybir.dt.bfloat16
I32 = mybir.dt.int32
AF = mybir.ActivationFunctionType
ALU = mybir.AluOpType
AX = mybir.AxisListType

NCORES = 8
SEM_EPOCH = 30000
NSLOT = 16


class Buf:
    __slots__ = ("last_w", "readers", "name", "psum")

    def __init__(self, name="", psum=False):
        self.last_w = None
        self.readers = []
        self.name = name
        self.psum = psum


class Op:
    __slots__ = ("eng", "emit", "deps", "is_dma", "slot", "slot_val", "cnt", "needed", "idx")


class Prog:
    ENGS = ["tensor", "vector", "scalar", "gpsimd", "sync"]

    def __init__(self, nc):
        self.nc = nc
        self.ops = {e: [] for e in self.ENGS}
        self.slot_count = {}
        self.all_ops = []
        self.out_dmas = []

    def op(self, eng, emit, r=(), w=(), dma=False, is_out=False):
        o = Op()
        o.eng = eng
        o.emit = emit
        o.is_dma = dma
        o.needed = False
        o.cnt = 0
        o.idx = len(self.all_ops)
        deps = {}
        for b in r:
            if b.last_w is not None:
                deps[b.last_w.idx] = b.last_w
            if b.psum:
                for rd in b.readers:
                    if rd.eng != eng:
                        deps[rd.idx] = rd
        for b in w:
            if b.last_w is not None:
                deps[b.last_w.idx] = b.last_w
            for rd in b.readers:
                deps[rd.idx] = rd
        for b in r:
            b.readers.append(o)
        for b in w:
            b.last_w = o
            b.readers = []
        deps.pop(o.idx, None)
        if eng == "tensor":
            o.deps = [d for d in deps.values() if d.eng != "tensor"]
        else:
            o.deps = list(deps.values())
        if dma:
            key = eng
            n = self.slot_count.get(key, 0)
            self.slot_count[key] = n + 1
            o.slot = (key, n % NSLOT)
            o.slot_val = 16 * (n // NSLOT + 1)
        self.ops[eng].append(o)
        self.all_ops.append(o)
        if is_out:
            self.out_dmas.append(o)
        return o

    def dma(self, out, in_, r=(), w=(), eng="sync", is_out=False, **kw):
        return self.op(eng, lambda e: e.dma_start(out=out, in_=in_, **kw), r=r, w=w,
                       dma=True, is_out=is_out)

    def mm(self, out, lhsT, rhs, start=True, stop=True, r=(), w=(), nochk=False):
        if nochk:
            return self.op("tensor", lambda e: e.matmul(out, lhsT, rhs, start=start, stop=stop,
                                                        skip_group_check=True), r=r, w=w)
        return self.op("tensor", lambda e: e.matmul(out, lhsT, rhs, start=start, stop=stop),
                       r=r, w=w)

    def tr(self, out, in_, ident, r=(), w=()):
        return self.op("tensor", lambda e: e.transpose(out, in_, ident), r=r, w=w)

    def act(self, out, in_, func, r=(), w=(), **kw):
        return self.op("scalar", lambda e: e.activation(out=out, in_=in_, func=func, **kw),
                       r=r, w=w)

    def tt(self, out, in0, in1, op, r=(), w=(), eng="vector"):
        return self.op(eng, lambda e: e.tensor_tensor(out=out, in0=in0, in1=in1, op=op),
                       r=r, w=w)

    def ts(self, out, in0, s1, op0, s2=None, op1=None, r=(), w=(), eng="vector", **kw):
        if op1 is None:
            return self.op(eng, lambda e: e.tensor_scalar(out=out, in0=in0, scalar1=s1,
                                                          scalar2=None, op0=op0, **kw), r=r, w=w)
        return self.op(eng, lambda e: e.tensor_scalar(out=out, in0=in0, scalar1=s1, scalar2=s2,
                                                      op0=op0, op1=op1, **kw), r=r, w=w)

    def stt(self, out, in0, scalar, in1, op0, op1, r=(), w=(), eng="vector"):
        return self.op(eng, lambda e: e.scalar_tensor_tensor(out=out, in0=in0, scalar=scalar,
                                                             in1=in1, op0=op0, op1=op1), r=r, w=w)

    def copy(self, out, in_, r=(), w=(), eng="vector"):
        if eng == "scalar":
            return self.op(eng, lambda e: e.copy(out=out, in_=in_), r=r, w=w)
        return self.op(eng, lambda e: e.tensor_copy(out=out, in_=in_), r=r, w=w)

    def memset(self, ap, val, w=(), eng="vector"):
        return self.op(eng, lambda e: e.memset(ap, val), w=w)

    def emit(self, stack):
        nc = self.nc
        for o in self.all_ops:
            for d in o.deps:
                d.needed = True
        sems = {}
        for e in self.ENGS:
            if e == "sync":
                continue
            c = 0
            for o in self.ops[e]:
                if o.is_dma:
                    continue
                if o.needed:
                    c += 1
                    o.cnt = c
            nsem = (c + SEM_EPOCH - 1) // SEM_EPOCH
            sems[e] = [stack.enter_context(nc.semaphore(f"s_{e}_{i}")) for i in range(max(nsem, 1))]
        slot_sems = {}
        for key, n in self.slot_count.items():
            for s in range(min(n, NSLOT)):
                slot_sems[(key, s)] = stack.enter_context(nc.semaphore(f"d_{key}_{s}"))
        block = stack.enter_context(nc.Block())
        out_dmas = self.out_dmas

        def run(engname, eng):
            seen = {}

            def wait_for(d):
                if d.is_dma:
                    k = ("dma",) + d.slot
                    if seen.get(k, 0) >= d.slot_val:
                        return
                    seen[k] = d.slot_val
                    eng.wait_ge(slot_sems[d.slot], d.slot_val)
                else:
                    ep = (d.cnt - 1) // SEM_EPOCH
                    v = d.cnt - ep * SEM_EPOCH
                    k = (d.eng, ep)
                    if seen.get(k, 0) >= v:
                        return
                    seen[k] = v
                    eng.wait_ge(sems[d.eng][ep], v)

            for o in self.ops[engname]:
                for d in o.deps:
                    wait_for(d)
                if o.is_dma:
                    if o.slot_val > 16:
                        k = ("dma",) + o.slot
                        if seen.get(k, 0) < o.slot_val - 16:
                            seen[k] = o.slot_val - 16
                            eng.wait_ge(slot_sems[o.slot], o.slot_val - 16)
                    ins = o.emit(eng)
                    ins.then_inc(slot_sems[o.slot], 16)
                else:
                    ins = o.emit(eng)
                    if o.needed:
                        ep = (o.cnt - 1) // SEM_EPOCH
                        ins.then_inc(sems[engname][ep], 1)
            if engname == "sync":
                for o in out_dmas:
                    eng.wait_ge(slot_sems[o.slot], o.slot_val)
                for key, n in self.slot_count.items():
                    for s in range(min(n, NSLOT)):
                        last = 16 * ((n - 1 - s) // NSLOT + 1)
                        eng.wait_ge(slot_sems[(key, s)], last)

        @block.tensor
        def _(e):
            run("tensor", e)

        @block.vector
        def _(e):
            run("vector", e)

        @block.scalar
        def _(e):
            run("scalar", e)

        @block.gpsimd
        def _(e):
            run("gpsimd", e)

        @block.sync
        def _(e):
            run("sync", e)


class Ctx:
    def __init__(self):
        self.nc = bass.Bass("TRN2", target_bir_lowering=False)
        self.stack = ExitStack()
        self.P = Prog(self.nc)
        self.n = 0

    def sb(self, shape, dt=F32, name=None):
        self.n += 1
        return self.stack.enter_context(
            self.nc.sbuf_tensor(f"sb{self.n}_{name or ''}", list(shape), dt))

    def ps(self, shape, dt=F32, name=None):
        self.n += 1
        return self.stack.enter_context(
            self.nc.psum_tensor(f"ps{self.n}_{name or ''}", list(shape), dt))

    def din(self, name, shape, dt=F32):
        return self.nc.dram_tensor(name, list(shape), dt, kind="ExternalInput").ap()

    def dout(self, name, shape, dt=F32):
        return self.nc.dram_tensor(name, list(shape), dt, kind="ExternalOutput").ap()

    def finish(self):
        self.P.emit(self.stack)
        self.stack.close()
        return self.nc


def run_spmd(nc, in_maps):
    res = run_bass_kernel_spmd(nc, in_maps, core_ids=list(range(len(in_maps))))
    return res.results


D = 1024
KC = D // 128
EPS = 1e-6


def load_col_vec(c, vec_ap, n, name):
    P = c.P
    t = c.sb([128, n // 128], F32, name)
    b = Buf(name)
    P.dma(t[:, :], vec_ap.rearrange("(c p) -> p c", p=128), w=[b], allow_slow_non_contiguous=True)
    return t, b


def fm_rmsnorm(c, xT, xb, g, gb, hT, hb, ones, onesb, ps, psb, tmp, tmpb, nt, kc=KC, dim=D,
               sq=None, sqb=None):
    P = c.P
    for ch in range(kc):
        P.act(sq[:, ch, :nt], xT[:, ch, :nt], AF.Square, r=[xb], w=[sqb])
    for ch in range(kc):
        P.mm(ps[:, :nt], ones[:, :], sq[:, ch, :nt], start=(ch == 0), stop=(ch == kc - 1),
             r=[sqb, onesb], w=[psb])
    P.ts(tmp[:, :nt], ps[:, :nt], 1.0 / dim, ALU.mult, EPS, ALU.add, r=[psb], w=[tmpb])
    P.act(tmp[:, :nt], tmp[:, :nt], AF.Sqrt, r=[tmpb], w=[tmpb])
    P.op("vector", lambda e: e.reciprocal(out=tmp[:, :nt], in_=tmp[:, :nt]), r=[tmpb], w=[tmpb])
    for ch in range(kc):
        P.stt(hT[:, ch, :nt], xT[:, ch, :nt], g[:, ch:ch + 1], tmp[:, :nt], ALU.mult, ALU.mult,
              r=[xb, gb, tmpb], w=[hb])


IN_COLS = 2336


def build_phase_a(nt_core, tb=512):
    c = Ctx()
    P = c.P
    xT_d = c.din("xT", [D, nt_core])
    w_d = c.din("w_in", [D, IN_COLS])
    g_d = c.din("g", [D])
    zT_d = c.dout("zT", [IN_COLS, nt_core])

    g, gb = load_col_vec(c, g_d, D, "g_sb")
    ones = c.sb([128, 128], F32, "ones")
    onesb = Buf()
    P.memset(ones[:, :], 1.0, w=[onesb])
    W = c.sb([128, KC, IN_COLS], BF16, "W")
    Wb = Buf()
    stg = [c.sb([128, IN_COLS], F32, f"stg{i}") for i in range(2)]
    stgb = [Buf(), Buf()]
    for ch in range(KC):
        s = ch % 2
        P.dma(stg[s][:, :], w_d[ch * 128:(ch + 1) * 128, :], w=[stgb[s]])
        P.copy(W[:, ch, :], stg[s][:, :], r=[stgb[s]], w=[Wb], eng="gpsimd")
    xT = [c.sb([128, KC, tb], F32, f"xT{i}") for i in range(2)]
    xb = [Buf(), Buf()]
    sq = c.sb([128, KC, tb], F32, "sq")
    sqb = Buf()
    hT = [c.sb([128, KC, tb], BF16, f"hT{i}") for i in range(2)]
    hb = [Buf(), Buf()]
    tmp = c.sb([128, tb], F32, "tmp")
    tmpb = Buf()
    ps_ss = c.ps([128, tb], F32, "ps_ss")
    ps_ssb = Buf(psum=True)
    psz = [c.ps([128, tb], F32, f"psz{i}") for i in range(4)]
    pszb = [Buf(psum=True) for _ in range(4)]
    zo = [c.sb([128, tb], F32, f"zo{i}") for i in range(4)]
    zob = [Buf() for _ in range(4)]
    nblk = nt_core // tb
    ncb = (IN_COLS + 127) // 128
    k = 0
    for blk in range(nblk):
        s = blk % 2
        t0 = blk * tb
        P.dma(xT[s][:, :, :], xT_d[:, t0:t0 + tb].rearrange("(c p) t -> p c t", p=128), w=[xb[s]])
        fm_rmsnorm(c, xT[s], xb[s], g, gb, hT[s], hb[s], ones, onesb, ps_ss, ps_ssb, tmp, tmpb, tb,
                   sq=sq, sqb=sqb)
        for cb in range(ncb):
            c0 = cb * 128
            cw = min(128, IN_COLS - c0)
            q = k % 4
            k += 1
            for ch in range(KC):
                P.mm(psz[q][:cw, :], W[:, ch, c0:c0 + cw], hT[s][:, ch, :], start=(ch == 0),
                     stop=(ch == KC - 1), r=[Wb, hb[s]], w=[pszb[q]])
            P.copy(zo[q][:cw, :], psz[q][:cw, :], r=[pszb[q]], w=[zob[q]],
                   eng=("scalar" if cb % 2 else "vector"))
            P.dma(zT_d[c0:c0 + cw, t0:t0 + tb], zo[q][:cw, :], r=[zob[q]], is_out=True,
                  eng="gpsimd")
    return c.finish()


import math
QK_NOPE, QK_ROPE, V_HEAD, Q_LORA, KV_LORA = 64, 32, 64, 256, 128
HD = QK_NOPE + QK_ROPE
ROPE_THETA = 10000.0


def mla_consts():
    invf = np.zeros((128, 1), np.float32)
    f = (ROPE_THETA ** (-np.arange(0, QK_ROPE, 2, dtype=np.float32) / QK_ROPE)).astype(np.float32)
    invf[64:80, 0] = f
    invf[80:96, 0] = f
    pa = np.zeros((32, 96), np.float32)
    pb = np.zeros((32, 96), np.float32)
    for i in range(32):
        pa[i, 64 + i] = 1.0
    for i in range(16):
        pb[i, 80 + i] = 1.0
        pb[16 + i, 64 + i] = -1.0
    return {"invf": invf, "placeA": pa, "placeB": pb}


def emit_mla(c, T, NB, zm_d, pos_d, qn_d, kvn_d, qup_d, kvupk_d, kvupv_d, invf_d, pa_d, pb_d, o_d):
    P = c.P
    TB = 512
    scale = float(HD) ** -0.5
    ones = c.sb([128, 128], F32, "m_ones")
    onesb = Buf()
    P.memset(ones[:, :], 1.0, w=[onesb])
    invf = c.sb([128, 1], F32, "m_invf")
    invfb = Buf()
    P.dma(invf[:, :], invf_d[:, :], w=[invfb])
    plf = c.sb([32, 2, 96], F32, "m_plf")
    plfb = Buf()
    P.dma(plf[:, 0, :], pa_d[:, :], w=[plfb])
    P.dma(plf[:, 1, :], pb_d[:, :], w=[plfb])
    pl = c.sb([32, 2, 96], BF16, "m_pl")
    plb = Buf()
    P.copy(pl[:, :, :], plf[:, :, :], r=[plfb], w=[plb])
    qg, qgb = load_col_vec(c, qn_d, Q_LORA, "m_qg")
    kg, kgb = load_col_vec(c, kvn_d, KV_LORA, "m_kg")
    wq = c.sb([128, 2, 96], F32, "m_wq")
    wqb = Buf()
    P.dma(wq[:, :, :], qup_d.rearrange("(c p) m -> p c m", p=128), w=[wqb])
    A = c.sb([128, 2, 96], BF16, "m_A")
    Bm = c.sb([128, 2, 96], BF16, "m_B")
    Ab = Buf()
    Bb = Buf()
    for ch in range(2):
        P.ts(wq[:, ch, :], wq[:, ch, :], qg[:, ch:ch + 1], ALU.mult, r=[wqb, qgb], w=[wqb])
    P.copy(A[:, :, :], wq[:, :, :], r=[wqb], w=[Ab])
    P.memset(Bm[:, :, :], 0.0, w=[Bb])
    P.ts(Bm[:, :, 64:80], wq[:, :, 80:96], -1.0, ALU.mult, r=[wqb], w=[Bb])
    P.copy(Bm[:, :, 80:96], wq[:, :, 64:80], r=[wqb], w=[Bb])
    wk = c.sb([128, 128], F32, "m_wk")
    wkb = Buf()
    P.dma(wk[:, 0:64], kvupk_d[:, :], w=[wkb])
    P.dma(wk[:, 64:128], kvupv_d[:, :], w=[wkb])
    P.ts(wk[:, :], wk[:, :], kg[:, 0:1], ALU.mult, r=[wkb, kgb], w=[wkb])
    KU = c.sb([128, 96], BF16, "m_KU")
    VU = c.sb([128, 64], BF16, "m_VU")
    KUb = Buf()
    VUb = Buf()
    P.memset(KU[:, :], 0.0, w=[KUb])
    P.copy(KU[:, 0:64], wk[:, 0:64], r=[wkb], w=[KUb])
    P.copy(VU[:, :], wk[:, 64:128], r=[wkb], w=[VUb])
    cos2 = c.sb([128, TB], F32, "m_cos")
    sin2 = c.sb([128, TB], F32, "m_sin")
    cosb = Buf()
    sinb = Buf()
    posi = c.sb([128, TB], I32, "m_posi")
    posib = Buf()
    ang = c.sb([128, TB], F32, "m_ang")
    angb = Buf()
    frc = c.sb([128, TB], F32, "m_frc")
    frcb = Buf()
    QT = c.sb([96, T], BF16, "m_QT")
    KT = c.sb([96, T], BF16, "m_KT")
    NTL = T // 128
    Vt = c.sb([128, NTL, 65], BF16, "m_Vt")
    nblk = T // TB
    QTb = [Buf() for _ in range(nblk)]
    KTb = [Buf() for _ in range(nblk)]
    Vtb = [Buf() for _ in range(nblk)]
    qd = [c.sb([128, 2, TB], F32, f"m_qd{i}") for i in range(2)]
    kvd = [c.sb([128, TB], F32, f"m_kvd{i}") for i in range(2)]
    kr = [c.sb([32, TB], F32, f"m_kr{i}") for i in range(2)]
    inb = [Buf(), Buf()]
    sq = c.sb([128, 3, TB], F32, "m_sq")
    sqb = Buf()
    rs = c.sb([128, 2, TB], F32, "m_rs")
    rsb = Buf()
    qn = c.sb([128, 2, TB], BF16, "m_qn")
    kvn = c.sb([128, TB], BF16, "m_kvn")
    krb16 = c.sb([32, TB], BF16, "m_krb")
    nb_ = Buf()
    t1 = c.sb([128, TB], F32, "m_t1")
    t2 = c.sb([128, TB], F32, "m_t2")
    t1b = Buf()
    t2b = Buf()
    NSB = 4
    pt = [c.sb([128, TB], BF16, f"m_pt{i}") for i in range(NSB)]
    ptb = [Buf() for _ in range(NSB)]
    osb = [c.sb([64, TB], F32, f"m_o{i}") for i in range(2)]
    osbb = [Buf(), Buf()]
    rcp = c.sb([128, TB], F32, "m_rcp")
    rcpb = Buf()
    bcs = c.sb([64, TB], F32, "m_bcs")
    bcsb = Buf()
    bank = [c.ps([128, 512], F32, f"m_bank{i}") for i in range(8)]
    bankb = [Buf(psum=True) for _ in range(8)]
    TWO_PI = 2.0 * math.pi

    for b in range(NB):
        for blk in range(nblk):
            s = blk % 2
            t0 = blk * TB
            ts_ = slice(t0, t0 + TB)
            P.dma(posi[:, :], pos_d[b, ts_].partition_broadcast(128), w=[posib])
            P.copy(ang[:, :], posi[:, :], r=[posib], w=[angb])
            P.ts(ang[:, :], ang[:, :], invf[:, 0:1], ALU.mult, r=[angb, invfb], w=[angb])
            for (dst, dstb, ph) in ((sin2, sinb, 0.5), (cos2, cosb, 0.75)):
                R_ = slice(64, 96)
                P.ts(dst[R_, :], ang[R_, :], 1.0 / TWO_PI, ALU.mult, ph, ALU.add, r=[angb], w=[dstb])
                P.copy(posi[R_, :], dst[R_, :], r=[dstb], w=[posib])
                P.copy(frc[R_, :], posi[R_, :], r=[posib], w=[frcb])
                P.tt(dst[R_, :], dst[R_, :], frc[R_, :], ALU.subtract, r=[dstb, frcb], w=[dstb])
                P.ts(frc[R_, :], dst[R_, :], 0.0, ALU.is_lt, r=[dstb], w=[frcb])
                P.tt(dst[R_, :], dst[R_, :], frc[R_, :], ALU.add, r=[dstb, frcb], w=[dstb])
                P.ts(dst[R_, :], dst[R_, :], TWO_PI, ALU.mult, -math.pi, ALU.add, r=[dstb], w=[dstb])
                P.ts(dst[R_, :], dst[R_, :], -math.pi, ALU.max, math.pi, ALU.min, r=[dstb], w=[dstb])
                P.act(dst[R_, :], dst[R_, :], AF.Sin, r=[dstb], w=[dstb])
            P.dma(qd[s][:, :, :], zm_d[b, 0:256, ts_].rearrange("(c p) t -> p c t", p=128),
                  w=[inb[s]])
            P.dma(kvd[s][:, :], zm_d[b, 256:384, ts_], w=[inb[s]])
            P.dma(kr[s][:, :], zm_d[b, 384:416, ts_], w=[inb[s]])
            for ch in range(2):
                P.act(sq[:, ch, :], qd[s][:, ch, :], AF.Square, r=[inb[s]], w=[sqb])
            P.act(sq[:, 2, :], kvd[s][:, :], AF.Square, r=[inb[s]], w=[sqb])
            P.mm(bank[0][:, :], ones[:, :], sq[:, 0, :], start=True, stop=False, r=[sqb, onesb],
                 w=[bankb[0]])
            P.mm(bank[0][:, :], ones[:, :], sq[:, 1, :], start=False, stop=True, r=[sqb, onesb],
                 w=[bankb[0]])
            P.mm(bank[1][:, :], ones[:, :], sq[:, 2, :], r=[sqb, onesb], w=[bankb[1]])
            P.ts(rs[:, 0, :], bank[0][:, :], 1.0 / Q_LORA, ALU.mult, EPS, ALU.add, r=[bankb[0]],
                 w=[rsb])
            P.ts(rs[:, 1, :], bank[1][:, :], 1.0 / KV_LORA, ALU.mult, EPS, ALU.add, r=[bankb[1]],
                 w=[rsb])
            P.act(rs[:, :, :], rs[:, :, :], AF.Sqrt, r=[rsb], w=[rsb])
            P.op("vector", lambda e: e.reciprocal(out=rs[:, :, :], in_=rs[:, :, :]), r=[rsb], w=[rsb])
            for ch in range(2):
                P.tt(qn[:, ch, :], qd[s][:, ch, :], rs[:, 0, :], ALU.mult, r=[inb[s], rsb], w=[nb_])
            P.tt(kvn[:, :], kvd[s][:, :], rs[:, 1, :], ALU.mult, r=[inb[s], rsb], w=[nb_])
            P.copy(krb16[:, :], kr[s][:, :], r=[inb[s]], w=[nb_], eng="gpsimd")
            for ch in range(2):
                P.mm(bank[2][:96, :], A[:, ch, :], qn[:, ch, :], start=(ch == 0), stop=(ch == 1),
                     r=[Ab, nb_], w=[bankb[2]])
            for ch in range(2):
                P.mm(bank[3][:96, :], Bm[:, ch, :], qn[:, ch, :], start=(ch == 0), stop=(ch == 1),
                     r=[Bb, nb_], w=[bankb[3]])
            P.mm(bank[4][:96, :], KU[:, :], kvn[:, :], start=True, stop=False, r=[KUb, nb_],
                 w=[bankb[4]])
            P.mm(bank[4][:96, :], pl[:, 0, :], krb16[:, :], start=False, stop=True, r=[plb, nb_],
                 w=[bankb[4]])
            P.mm(bank[5][:96, :], pl[:, 1, :], krb16[:, :], r=[plb, nb_], w=[bankb[5]])
            for j in range(4):
                P.mm(bank[6][:, j * 64:(j + 1) * 64], kvn[:, j * 128:(j + 1) * 128], VU[:, :],
                     r=[VUb, nb_], w=[bankb[6]])
            P.copy(QT[0:64, ts_], bank[2][0:64, :], r=[bankb[2]], w=[QTb[blk]], eng="scalar")
            P.tt(t1[64:96, :], bank[3][64:96, :], sin2[64:96, :], ALU.mult, r=[bankb[3], sinb],
                 w=[t1b])
            P.tt(t2[64:96, :], bank[2][64:96, :], cos2[64:96, :], ALU.mult, r=[bankb[2], cosb],
                 w=[t2b])
            P.tt(QT[64:96, ts_], t1[64:96, :], t2[64:96, :], ALU.add, r=[t1b, t2b], w=[QTb[blk]],
                 eng="gpsimd")
            P.copy(KT[0:64, ts_], bank[4][0:64, :], r=[bankb[4]], w=[KTb[blk]], eng="scalar")
            P.tt(t1[64:96, :], bank[5][64:96, :], sin2[64:96, :], ALU.mult, r=[bankb[5], sinb],
                 w=[t1b])
            P.tt(t2[64:96, :], bank[4][64:96, :], cos2[64:96, :], ALU.mult, r=[bankb[4], cosb],
                 w=[t2b])
            P.tt(KT[64:96, ts_], t1[64:96, :], t2[64:96, :], ALU.add, r=[t1b, t2b], w=[KTb[blk]],
                 eng="gpsimd")
            P.copy(Vt[:, blk * 4:(blk + 1) * 4, 0:64],
                   bank[6][:, 0:256].rearrange("p (j v) -> p j v", v=64), r=[bankb[6]],
                   w=[Vtb[blk]])
            P.memset(Vt[:, blk * 4:(blk + 1) * 4, 64:65], 1.0, w=[Vtb[blk]], eng="gpsimd")
        for qb in range(nblk):
            qs = slice(qb * TB, (qb + 1) * TB)
            accb = bankb[7]
            acc = bank[7]
            def s_exp(kt):
                s = kt % NSB
                kb = kt // 4
                P.mm(bank[s][:, :], KT[:, kt * 128:(kt + 1) * 128], QT[:, qs], r=[KTb[kb], QTb[qb]],
                     w=[bankb[s]])
                P.act(pt[s][:, :], bank[s][:, :], AF.Exp, scale=scale, r=[bankb[s]], w=[ptb[s]])

            LOOK = NSB - 1
            for kt in range(min(LOOK, NTL)):
                s_exp(kt)
            for kt in range(NTL):
                if kt + LOOK < NTL:
                    s_exp(kt + LOOK)
                s = kt % NSB
                kb = kt // 4
                P.mm(acc[0:65, :], Vt[:, kt, :], pt[s][:, :], start=(kt == 0), stop=(kt == NTL - 1),
                     r=[ptb[s], Vtb[kb]], w=[accb])
            so = qb % 2
            P.op("vector", lambda e: e.reciprocal(out=rcp[64:65, :], in_=acc[64:65, :]),
                 r=[accb], w=[rcpb])
            P.mm(bank[6][0:64, :], ones[64:65, 0:64], rcp[64:65, :], r=[onesb, rcpb], w=[bankb[6]])
            P.copy(bcs[0:64, :], bank[6][0:64, :], r=[bankb[6]], w=[bcsb], eng="scalar")
            P.tt(osb[so][0:64, :], acc[0:64, :], bcs[0:64, :], ALU.mult, r=[accb, bcsb], w=[osbb[so]])
            P.dma(o_d[b, :, qb * TB:(qb + 1) * TB], osb[so][0:64, :], r=[osbb[so]], is_out=True,
                  eng="gpsimd")


def build_mla(T, NB):
    c = Ctx()
    zm_d = c.din("zm", [NB, 416, T])
    pos_d = c.din("pos", [NB, T], I32)
    qn_d = c.din("q_norm", [Q_LORA])
    kvn_d = c.din("kv_norm", [KV_LORA])
    qup_d = c.din("q_up", [Q_LORA, 96])
    kk_d = c.din("kv_up_k", [KV_LORA, 64])
    kv_d = c.din("kv_up_v", [KV_LORA, 64])
    invf_d = c.din("invf", [128, 1])
    pa_d = c.din("placeA", [32, 96])
    pb_d = c.din("placeB", [32, 96])
    o_d = c.dout("o", [NB, 64, T])
    emit_mla(c, T, NB, zm_d, pos_d, qn_d, kvn_d, qup_d, kk_d, kv_d, invf_d, pa_d, pb_d, o_d)
    return c.finish()


RW_N = 64
LOGW_SCALE = -0.6065306597126334
GN_EPS = 64e-5
RWC = 576


def rwkv_consts():
    idx = np.arange(128)
    s = idx[:, None]
    t = idx[None, :]
    le = (s <= t).astype(np.float32)
    lt = (s < t).astype(np.float32)
    gt = (s > t).astype(np.float32)
    lem = (idx <= 63).astype(np.float32)[:, None]
    f = {}
    f["ME1"] = le - lem
    f["ME2"] = lt - lem
    msel = np.zeros((128, 2), np.float32)
    msel[:, 0] = lem[:, 0]
    msel[:, 1] = 1.0
    f["MSEL"] = msel
    f["ME4"] = gt
    f["MASK4"] = np.concatenate([le, lt, le, lt], axis=1)
    f["STRT"] = gt.copy()
    out = {}
    for d in range(2):
        for k, v in f.items():
            if d == 0:
                m = v
            else:
                m = v[::-1]
                if k == "MASK4":
                    m = np.concatenate([m[:, i * 128:(i + 1) * 128][:, ::-1] for i in range(4)], 1)
                elif k != "MSEL":
                    m = m[:, ::-1]
            out[f"{k}{d}"] = np.ascontiguousarray(m, dtype=np.float32)
    out["ident"] = np.eye(128, dtype=np.float32)
    return out


RW_CONST_SHAPES = {"ME1": [128, 128], "ME2": [128, 128], "MSEL": [128, 2], "ME4": [128, 128],
                   "MASK4": [128, 512], "STRT": [128, 128]}


def rwkv_host_params(h, mu, w0, w_up, a0, a_up, g_up, k_k, k_a, r_k, lnx_w, lnx_b):
    hs = slice(h * 64, (h + 1) * 64)
    o3 = 1536
    cols = np.concatenate([np.arange(h * 64, h * 64 + 64), 512 + np.arange(h * 64, h * 64 + 64),
                           1024 + np.arange(h * 64, h * 64 + 64),
                           o3 + np.arange(0, 64), o3 + 128 + np.arange(0, 64),
                           o3 + 64 + np.arange(0, 64), o3 + 128 + 64 + np.arange(0, 64),
                           o3 + 256 + np.arange(0, 128)])
    p = {"rw_cols": cols, "mu": np.ascontiguousarray(mu[cols])}
    for d in range(2):
        bd = np.zeros((128, 128), np.float32)
        bd[0:64, 0:64] = w_up[d][:, hs]
        bd[64:128, 64:128] = a_up[d][:, hs]
        p[f"up_bd{d}"] = bd
        p[f"bias{d}"] = np.concatenate([w0[d, hs], a0[d, hs]]).astype(np.float32)
    p["g_up"] = np.ascontiguousarray(g_up[:, hs])
    p["k_k"] = np.ascontiguousarray(k_k[hs])
    p["k_a"] = np.ascontiguousarray(k_a[hs])
    p["r_k"] = np.ascontiguousarray(r_k[h])
    p["lnx_w"] = np.ascontiguousarray(lnx_w[hs])
    p["lnx_b"] = np.ascontiguousarray(lnx_b[hs])
    return p


def emit_rwkv(c, T, NB, zr_d, pd, cd, y_d):
    P = c.P
    nc = c.nc
    NT = T // 128
    def load_const(name, shape):
        t = c.sb(shape, F32, "k_" + name)
        b = Buf()
        P.dma(t[:, :], cd[name][:, :], w=[b])
        return t, b
    ident, identb = load_const("ident", [128, 128])
    K = {}
    for d in range(2):
        for k, shp in RW_CONST_SHAPES.items():
            K[(k, d)] = load_const(f"{k}{d}", shp)

    def bcast(name, n, scale=None):
        t = c.sb([128, n], F32, "b_" + name)
        b = Buf()
        P.dma(t[:, :], pd[name].partition_broadcast(128), w=[b])
        if scale is not None:
            P.ts(t[:, :], t[:, :], scale, ALU.mult, r=[b], w=[b])
        return t, b
    mu_b, mu_bb = bcast("mu", RWC)
    kkp_b, kkp_bb = bcast("k_k", 64)
    ka_b, ka_bb = bcast("k_a", 64)
    rk_b, rk_bb = bcast("r_k", 64, 0.5)
    lnw_b, lnw_bb = bcast("lnx_w", 64)
    lnb_b, lnb_bb = bcast("lnx_b", 64)
    bias_b = [bcast(f"bias{d}", 128) for d in range(2)]
    up_bd = [load_const_p(c, pd[f"up_bd{d}"], [128, 128], f"up_bd{d}") for d in range(2)]
    g_up = load_const_p(c, pd["g_up"], [128, 64], "g_up")
    osp = nc.dram_tensor("rw_osp", [NB, 2, T, 64], F32, kind="Internal").ap()
    vsp = nc.dram_tensor("rw_vsp", [NB, T, 64], F32, kind="Internal").ap()
    gsp = nc.dram_tensor("rw_gsp", [NB, T, 64], F32, kind="Internal").ap()
    bsp = nc.dram_tensor("rw_bsp", [NB, 2, T], F32, kind="Internal").ap()
    spb = [[[Buf() for _ in range(NT)] for _ in range(2)] for _ in range(NB)]
    banks = [c.ps([128, 512], F32, f"rw_bank{i}") for i in range(8)]
    bankb = [Buf(psum=True) for _ in range(8)]

    def pass_gen(b, d, pi):
        tg = f"_{b}{d}"
        bA, bB = banks[2 * pi], banks[2 * pi + 1]
        bAb, bBb = bankb[2 * pi], bankb[2 * pi + 1]
        NCOL = 448 if d == 0 else 320
        zc = c.sb([128, 448], F32, "zc" + tg)
        zp = c.sb([128, 448], F32, "zp" + tg)
        zn = c.sb([128, 448], F32, "zn" + tg)
        zm = c.sb([128, 448], F32, "zm" + tg)
        mt = c.sb([128, 448], F32, "mt" + tg)
        zinb, zmb, mtb = Buf(), Buf(), Buf()
        mub = c.sb([128, 448], F32, "mub" + tg)
        mubb = Buf()
        P.copy(mub[:, 0:192], mu_b[:, 0:192], r=[mu_bb], w=[mubb])
        if d == 0:
            P.copy(mub[:, 192:448], mu_b[:, 192:320], r=[mu_bb], w=[mubb]) if False else None
            P.copy(mub[:, 192:320], mu_b[:, 192:320], r=[mu_bb], w=[mubb])
            P.copy(mub[:, 320:448], mu_b[:, 448:576], r=[mu_bb], w=[mubb])
        else:
            P.copy(mub[:, 192:320], mu_b[:, 320:448], r=[mu_bb], w=[mubb])
        twa = c.sb([128, 128], F32, "twa" + tg)
        sgd = c.sb([128, 128], F32, "sgd" + tg)
        twab, sgdb = Buf(), Buf()
        wa = c.sb([128, 128], F32, "wa" + tg)
        sig = c.sb([128, 128], F32, "sig" + tg)
        wab, sigb = Buf(), Buf()
        gt_ = c.sb([128, 64], F32, "gt" + tg)
        gtb = Buf()
        logw = c.sb([128, 64], F32, "logw" + tg)
        logwb = Buf()
        kk0 = c.sb([128, 64], F32, "kk0" + tg)
        kk = c.sb([128, 64], F32, "kk" + tg)
        junk = c.sb([128, 64], F32, "junk" + tg)
        ss = c.sb([128, 1], F32, "ss" + tg)
        kk0b, kkb, junkb, ssb = Buf(), Buf(), Buf(), Buf()
        uu = c.sb([128, 64], F32, "uu" + tg)
        kdir = c.sb([128, 64], F32, "kdir" + tg)
        nakk = c.sb([128, 64], F32, "nakk" + tg)
        uub, kdirb, nakkb = Buf(), Buf(), Buf()
        bt1 = c.sb([128, 64], F32, "bt1" + tg)
        bs = c.sb([128, 1], F32, "bs" + tg)
        bt1b, bsb = Buf(), Buf()
        e12 = c.sb([64, 256], F32, "e12" + tg)
        e3 = c.sb([64, 128], F32, "e3" + tg)
        ecc = c.sb([64, 2], F32, "ecc" + tg)
        e4 = c.sb([128, 64], F32, "e4" + tg)
        e12b, e3b, eccb, e4b = Buf(), Buf(), Buf(), Buf()
        FM = c.sb([64, 256], F32, "FM" + tg)
        FMu = c.sb([64, 256], F32, "FMu" + tg)
        AK = c.sb([64, 256], F32, "AK" + tg)
        FMb, FMub, AKb = Buf(), Buf(), Buf()
        AH = c.sb([128, 64], F32, "AH" + tg)
        KH = c.sb([128, 64], F32, "KH" + tg)
        AHb, KHb = Buf(), Buf()
        LM = c.sb([128, 512], F32, "LM" + tg)
        L1 = c.sb([128, 128], F32, "L1" + tg)
        LMb, L1b = Buf(), Buf()
        AB = [c.sb([128, 256], F32, f"AB{i}" + tg) for i in range(2)]
        ABb = [Buf(), Buf()]
        Wt = [c.sb([128, 128], F32, f"W{i}" + tg) for i in range(2)]
        Wtb = [Buf(), Buf()]
        H = [c.sb([64, 64], F32, f"H{i}" + tg) for i in range(2)]
        Hb = [Buf(), Buf()]
        Xs = c.sb([128, 64], F32, "Xs" + tg)
        Us = c.sb([128, 64], F32, "Us" + tg)
        Os = c.sb([128, 64], F32, "Os" + tg)
        Xsb, Usb, Osb = Buf(), Buf(), Buf()
        ME1, ME1b = K[("ME1", d)]
        ME2, ME2b = K[("ME2", d)]
        MSEL, MSELb = K[("MSEL", d)]
        ME4, ME4b = K[("ME4", d)]
        MASK4, MASK4b = K[("MASK4", d)]
        STRT, STRTb = K[("STRT", d)]
        ubd, ubdb = up_bd[d]
        bia, biab = bias_b[d]
        P.memset(H[0][:, :], 0.0, w=[Hb[0]])
        hc = 0
        lo_src = 192 if d == 0 else 320
        for step in range(NT):
            ti = step if d == 0 else NT - 1 - step
            r0 = ti * 128
            for (dst, off) in ((zp, 0), (zc, 1), (zn, 2)):
                P.dma(dst[:, 0:192], zr_d[b, r0 + off:r0 + off + 128, 0:192], w=[zinb])
                P.dma(dst[:, 192:320], zr_d[b, r0 + off:r0 + off + 128, lo_src:lo_src + 128],
                      w=[zinb])
                if d == 0:
                    P.dma(dst[:, 320:448], zr_d[b, r0 + off:r0 + off + 128, 448:576], w=[zinb])
            yield
            N_ = NCOL
            P.tt(mt[:, :N_], zp[:, :N_], zn[:, :N_], ALU.add, r=[zinb], w=[mtb], eng="gpsimd")
            P.stt(mt[:, :N_], mt[:, :N_], 0.5, zc[:, :N_], ALU.mult, ALU.subtract, r=[mtb, zinb],
                  w=[mtb])
            P.tt(mt[:, :N_], mt[:, :N_], mub[:, :N_], ALU.mult, r=[mtb, mubb], w=[mtb], eng="gpsimd")
            P.tt(zm[:, :N_], zc[:, :N_], mt[:, :N_], ALU.add, r=[zinb, mtb], w=[zmb])
            rr, kc, vv = zm[:, 0:64], zm[:, 64:128], zm[:, 128:192]
            if d == 0:
                P.dma(vsp[b, r0:r0 + 128, :], vv, r=[zmb], w=[spb[b][0][ti]], eng="gpsimd")
            yield
            P.tr(bA[:, 0:128], zm[:, 192:320], ident[:, :], r=[zmb, identb], w=[bAb])
            if d == 0:
                P.tr(bA[:, 128:256], zm[:, 320:448], ident[:, :], r=[zmb, identb], w=[bAb])
            P.act(twa[0:64, :], bA[0:64, 0:128], AF.Tanh, r=[bAb], w=[twab])
            P.copy(twa[64:128, :], bA[64:128, 0:128], r=[bAb], w=[twab])
            if d == 0:
                P.act(sgd[:, :], bA[:, 128:256], AF.Sigmoid, r=[bAb], w=[sgdb])
            yield
            P.mm(bB[:, 0:128], twa[:, :], ubd[:, :], r=[twab, ubdb], w=[bBb])
            if d == 0:
                P.mm(bB[:, 128:192], sgd[:, :], g_up[0][:, :], r=[sgdb, g_up[1]], w=[bBb])
            P.tt(wa[:, :], bB[:, 0:128], bia[:, :], ALU.add, r=[bBb, biab], w=[wab])
            if d == 0:
                P.copy(gt_[:, :], bB[:, 128:192], r=[bBb], w=[gtb], eng="scalar")
                P.dma(gsp[b, r0:r0 + 128, :], gt_[:, :], r=[gtb], w=[spb[b][0][ti]], eng="gpsimd")
            P.act(sig[:, :], wa[:, :], AF.Sigmoid, r=[wab], w=[sigb])
            P.ts(logw[:, :], sig[:, 0:64], LOGW_SCALE, ALU.mult, r=[sigb], w=[logwb])
            aa = sig[:, 64:128]
            P.tt(kk0[:, :], kc, kkp_b[:, :], ALU.mult, r=[zmb, kkp_bb], w=[kk0b], eng="gpsimd")
            P.act(junk[:, :], kk0[:, :], AF.Square, r=[kk0b], w=[junkb, ssb], accum_out=ss[:, 0:1])
            P.act(ss[:, :], ss[:, :], AF.Sqrt, r=[ssb], w=[ssb])
            P.ts(ss[:, :], ss[:, :], 1e-12, ALU.max, r=[ssb], w=[ssb])
            P.op("vector", lambda e, ss=ss: e.reciprocal(out=ss[:, :], in_=ss[:, :]), r=[ssb], w=[ssb])
            P.ts(kk[:, :], kk0[:, :], ss[:, 0:1], ALU.mult, r=[kk0b, ssb], w=[kkb])
            P.stt(uu[:, :], aa, -1.0, ka_b[:, :], ALU.add, ALU.mult, r=[sigb, ka_bb], w=[uub])
            P.stt(kdir[:, :], uu[:, :], 1.0, kc, ALU.add, ALU.mult, r=[uub, zmb], w=[kdirb])
            P.stt(nakk[:, :], aa, -1.0, kk[:, :], ALU.mult, ALU.mult, r=[sigb, kkb], w=[nakkb])
            P.tt(bt1[:, :], kdir[:, :], rr, ALU.mult, r=[kdirb, zmb], w=[bt1b], eng="gpsimd")
            P.tt(bt1[:, :], bt1[:, :], rk_b[:, :], ALU.mult, r=[bt1b, rk_bb], w=[bt1b], eng="gpsimd")
            P.op("vector", lambda e, bs=bs, bt1=bt1: e.reduce_sum(out=bs[:, :], in_=bt1[:, :],
                                                                  axis=AX.X), r=[bt1b], w=[bsb])
            P.dma(bsp[b, d, r0:r0 + 128].rearrange("(p o) -> p o", o=1), bs[:, :], r=[bsb],
                  w=[spb[b][d][ti]], eng="gpsimd")
            yield
            P.mm(bA[0:64, 0:128], logw[:, :], ME1[:, :], r=[logwb, ME1b], w=[bAb])
            P.mm(bA[0:64, 128:256], logw[:, :], ME2[:, :], r=[logwb, ME2b], w=[bAb])
            P.mm(bA[0:64, 256:258], logw[:, :], MSEL[:, :], r=[logwb, MSELb], w=[bAb])
            P.mm(bA[:, 260:324], ME4[:, :], logw[:, :], r=[logwb, ME4b], w=[bAb])
            P.act(e12[:, :], bA[0:64, 0:256], AF.Exp, r=[bAb], w=[e12b])
            P.act(e3[:, :], bA[0:64, 0:128], AF.Exp, r=[bAb], w=[e3b], scale=-1.0)
            P.act(ecc[:, :], bA[0:64, 256:258], AF.Exp, r=[bAb], w=[eccb])
            P.act(e4[:, :], bA[:, 260:324], AF.Exp, r=[bAb], w=[e4b])
            yield
            P.tr(bB[0:64, 0:128], rr, ident[:, :], r=[zmb, identb], w=[bBb])
            P.tr(bB[0:64, 128:256], kk[:, :], ident[:, :], r=[kkb, identb], w=[bBb])
            P.tr(bB[0:64, 256:384], nakk[:, :], ident[:, :], r=[nakkb, identb], w=[bBb])
            P.tr(bB[0:64, 384:512], kdir[:, :], ident[:, :], r=[kdirb, identb], w=[bBb])
            P.tt(FM[:, :], bB[0:64, 0:256], e12[:, :], ALU.mult, r=[bBb, e12b], w=[FMb])
            P.tt(AK[:, 0:128], bB[0:64, 256:384], e3[:, :], ALU.mult, r=[bBb, e3b], w=[AKb])
            P.tt(AK[:, 128:256], bB[0:64, 384:512], e3[:, :], ALU.mult, r=[bBb, e3b], w=[AKb])
            P.ts(FMu[:, :], FM[:, :], ecc[:, 0:1], ALU.mult, r=[FMb, eccb], w=[FMub])
            P.tt(AH[:, :], nakk[:, :], e4[:, :], ALU.mult, r=[nakkb, e4b], w=[AHb], eng="gpsimd")
            P.tt(KH[:, :], kdir[:, :], e4[:, :], ALU.mult, r=[kdirb, e4b], w=[KHb], eng="gpsimd")
            yield
            Rt, Bt = FM[:, 0:128], FM[:, 128:256]
            At, Kt = AK[:, 0:128], AK[:, 128:256]
            P.mm(bA[:, 0:256], Kt, FM[:, :], r=[AKb, FMb], w=[bAb])
            P.mm(bA[:, 256:512], At, FM[:, :], r=[AKb, FMb], w=[bAb])
            P.mm(bB[:, 0:128], Bt, At, r=[AKb, FMb], w=[bBb])
            P.tt(LM[:, :], bA[:, :], MASK4[:, :], ALU.mult, r=[bAb, MASK4b], w=[LMb])
            P.tt(L1[:, :], bB[:, 0:128], STRT[:, :], ALU.mult, r=[bBb, STRTb], w=[L1b])
            M2T, L2T, M1T, Nm = LM[:, 0:128], LM[:, 128:256], LM[:, 256:384], LM[:, 384:512]
            P.tt(Wt[0][:, :], Nm, ident[:, :], ALU.add, r=[LMb, identb], w=[Wtb[0]], eng="gpsimd")
            yield
            for j in range(1, 8):
                bk, bkb = (bA, bAb) if j % 2 else (bB, bBb)
                if j == 1:
                    Ap, Bp, rdp = L1[:, :], Nm, [L1b, LMb]
                else:
                    Ap, Bp, rdp = AB[(j - 1) % 2][:, 0:128], AB[(j - 1) % 2][:, 128:256], \
                        [ABb[(j - 1) % 2]]
                if j <= 6:
                    P.mm(bk[:, 0:128], Bp, Ap, r=rdp, w=[bkb])
                if j <= 5:
                    P.mm(bk[:, 128:256], Ap, Bp, r=rdp, w=[bkb])
                if j >= 2:
                    wi = (j - 2) % 2
                    P.mm(bk[:, 256:384], Ap, Wt[wi][:, :], r=rdp + [Wtb[wi]], w=[bkb])
                if j <= 6:
                    ncols = 256 if j <= 5 else 128
                    P.copy(AB[j % 2][:, 0:ncols], bk[:, 0:ncols], r=[bkb], w=[ABb[j % 2]],
                           eng="scalar")
                if j >= 2:
                    wi = (j - 2) % 2
                    P.tt(Wt[1 - wi][:, :], bk[:, 256:384], Wt[wi][:, :], ALU.add,
                         r=[bkb, Wtb[wi]], w=[Wtb[1 - wi]])
                yield
            W6, W6b = Wt[0], Wtb[0]
            Hc, Hcb = H[hc], Hb[hc]
            Hn, Hnb = H[1 - hc], Hb[1 - hc]
            Rtu, Btu = FMu[:, 0:128], FMu[:, 128:256]
            P.mm(bA[:, 0:64], L2T, vv, start=True, stop=False, r=[LMb, zmb], w=[bAb])
            P.mm(bA[:, 0:64], Btu, Hc[:, :], start=False, stop=True, r=[FMub, Hcb], w=[bAb])
            P.copy(Xs[:, :], bA[:, 0:64], r=[bAb], w=[Xsb])
            P.mm(bB[:, 0:64], W6[:, :], Xs[:, :], r=[W6b, Xsb], w=[bBb])
            P.copy(Us[:, :], bB[:, 0:64], r=[bBb], w=[Usb], eng="scalar")
            P.mm(bA[0:64, 64:128], KH[:, :], vv, start=True, stop=False, r=[KHb, zmb], w=[bAb])
            P.mm(bA[0:64, 64:128], AH[:, :], Us[:, :], start=False, stop=True, r=[AHb, Usb], w=[bAb])
            P.mm(bA[:, 128:192], M2T, vv, start=True, stop=False, r=[LMb, zmb], w=[bAb])
            P.mm(bA[:, 128:192], Rtu, Hc[:, :], start=False, stop=False, r=[FMub, Hcb], w=[bAb])
            P.mm(bA[:, 128:192], M1T, Us[:, :], start=False, stop=True, r=[LMb, Usb], w=[bAb])
            P.stt(Hn[:, :], Hc[:, :], ecc[:, 1:2], bA[0:64, 64:128], ALU.mult, ALU.add,
                  r=[Hcb, eccb, bAb], w=[Hnb])
            P.copy(Os[:, :], bA[:, 128:192], r=[bAb], w=[Osb], eng="scalar")
            P.dma(osp[b, d, r0:r0 + 128, :], Os[:, :], r=[Osb], w=[spb[b][d][ti]], eng="gpsimd")
            hc = 1 - hc
            yield

    gens = []
    pi = 0
    for b in range(NB):
        for d in range(2):
            gens.append(pass_gen(b, d, pi % 4))
            pi += 1
    grp = 4
    for g0 in range(0, len(gens), grp):
        live = gens[g0:g0 + grp]
        for k, g in enumerate(live):
            for _ in range(4 * k):
                next(g)
        while live:
            for g in list(live):
                try:
                    next(g)
                except StopIteration:
                    live.remove(g)
    TBK = min(NT, 16)
    of = c.sb([128, TBK, 64], F32, "ep_of")
    ob = c.sb([128, TBK, 64], F32, "ep_ob")
    vt = c.sb([128, TBK, 64], F32, "ep_v")
    gt2 = c.sb([128, TBK, 64], F32, "ep_g")
    b0 = c.sb([128, TBK], F32, "ep_b0")
    b1 = c.sb([128, TBK], F32, "ep_b1")
    st = c.sb([128, TBK], F32, "ep_st")
    sq2 = c.sb([128, TBK, 64], F32, "ep_sq")
    epb = Buf()
    for b in range(NB):
        for k0 in range(0, NT, TBK):
            rs_ = slice(k0 * 128, (k0 + TBK) * 128)
            deps = [spb[b][d][ti] for d in range(2) for ti in range(k0, k0 + TBK)]
            pat = "(j p) v -> p j v"
            P.dma(of[:, :, :], osp[b, 0, rs_, :].rearrange(pat, p=128), r=deps, w=[epb])
            P.dma(ob[:, :, :], osp[b, 1, rs_, :].rearrange(pat, p=128), r=deps, w=[epb])
            P.dma(vt[:, :, :], vsp[b, rs_, :].rearrange(pat, p=128), r=deps, w=[epb])
            P.dma(gt2[:, :, :], gsp[b, rs_, :].rearrange(pat, p=128), r=deps, w=[epb])
            P.dma(b0[:, :], bsp[b, 0, rs_].rearrange("(j p) -> p j", p=128), r=deps, w=[epb],
                  allow_slow_non_contiguous=True)
            P.dma(b1[:, :], bsp[b, 1, rs_].rearrange("(j p) -> p j", p=128), r=deps, w=[epb],
                  allow_slow_non_contiguous=True)
            E = [epb]
            bc = lambda t_: t_[:, :].unsqueeze(2).to_broadcast([128, TBK, 64])
            P.tt(of[:, :, :], of[:, :, :], ob[:, :, :], ALU.add, r=E, w=E)
            P.op("vector", lambda e: e.reduce_sum(out=st[:, :], in_=of[:, :, :], axis=AX.X), r=E, w=E)
            P.ts(st[:, :], st[:, :], 1.0 / 64, ALU.mult, r=E, w=E)
            P.tt(of[:, :, :], of[:, :, :], bc(st), ALU.subtract, r=E, w=E)
            P.tt(sq2[:, :, :], of[:, :, :], of[:, :, :], ALU.mult, r=E, w=E)
            P.op("vector", lambda e: e.reduce_sum(out=st[:, :], in_=sq2[:, :, :], axis=AX.X), r=E, w=E)
            P.ts(st[:, :], st[:, :], 1.0 / 64, ALU.mult, GN_EPS, ALU.add, r=E, w=E)
            P.act(st[:, :], st[:, :], AF.Sqrt, r=E, w=E)
            P.op("vector", lambda e: e.reciprocal(out=st[:, :], in_=st[:, :]), r=E, w=E)
            P.tt(of[:, :, :], of[:, :, :], bc(st), ALU.mult, r=E, w=E)
            P.tt(of[:, :, :], of[:, :, :], lnw_b[:, :].unsqueeze(1).to_broadcast([128, TBK, 64]),
                 ALU.mult, r=E + [lnw_bb], w=E)
            P.tt(of[:, :, :], of[:, :, :], lnb_b[:, :].unsqueeze(1).to_broadcast([128, TBK, 64]),
                 ALU.add, r=E + [lnb_bb], w=E)
            P.tt(b0[:, :], b0[:, :], b1[:, :], ALU.add, r=E, w=E)
            P.tt(vt[:, :, :], vt[:, :, :], bc(b0), ALU.mult, r=E, w=E)
            P.tt(of[:, :, :], of[:, :, :], vt[:, :, :], ALU.add, r=E, w=E)
            P.tt(of[:, :, :], of[:, :, :], gt2[:, :, :], ALU.mult, r=E, w=E)
            P.dma(y_d[b, rs_, :].rearrange(pat, p=128), of[:, :, :], r=E, is_out=True)


def load_const_p(c, ap, shape, name):
    t = c.sb(shape, F32, "p_" + name)
    b = Buf()
    c.P.dma(t[:, :], ap[:, :], w=[b])
    return t, b


RW_PARAM_SHAPES = {"mu": [RWC], "up_bd0": [128, 128], "up_bd1": [128, 128], "bias0": [128],
                   "bias1": [128], "g_up": [128, 64], "k_k": [64], "k_a": [64], "r_k": [64],
                   "lnx_w": [64], "lnx_b": [64]}


def build_rwkv(T, NB):
    c = Ctx()
    zr_d = c.din("zr", [NB, T + 2, RWC])
    pd = {k: c.din("rwp_" + k, shp) for k, shp in RW_PARAM_SHAPES.items()}
    cd = {"ident": c.din("rwc_ident", [128, 128])}
    for d in range(2):
        for k, shp in RW_CONST_SHAPES.items():
            cd[f"{k}{d}"] = c.din(f"rwc_{k}{d}", shp)
    y_d = c.dout("y_rw", [NB, T, 64])
    emit_rwkv(c, T, NB, zr_d, pd, cd, y_d)
    return c.finish()


def rwkv_in_map(h, z_rw, params):
    p = rwkv_host_params(h, *params)
    cols = p.pop("rw_cols")
    NB, T, _ = z_rw.shape
    zr = np.zeros((NB, T + 2, RWC), np.float32)
    zr[:, 1:T + 1, :] = z_rw[:, :, cols]
    m = {"zr": zr}
    for k, v in p.items():
        m["rwp_" + k] = np.ascontiguousarray(v, dtype=np.float32)
    for k, v in rwkv_consts().items():
        m["rwc_" + k] = v
    return m


NEXP = 16
CAP = 1024


def load_w_bf16(c, w_ap, kc, ncols, name, eng="gpsimd"):
    P = c.P
    W = c.sb([128, kc, ncols], BF16, name)
    Wb = Buf()
    stg = [c.sb([128, ncols], F32, f"{name}_stg{i}") for i in range(2)]
    stgb = [Buf(), Buf()]
    for ch in range(kc):
        s = ch % 2
        P.dma(stg[s][:, :], w_ap[ch * 128:(ch + 1) * 128, :], w=[stgb[s]])
        P.copy(W[:, ch, :], stg[s][:, :], r=[stgb[s]], w=[Wb], eng=eng)
    return W, Wb


def build_phase_c(nt_core, tb=512):
    c = Ctx()
    P = c.P
    xT_d = c.din("xT", [D, nt_core])
    yrw_d = c.din("yrwT", [512, nt_core])
    om_d = c.din("omT", [512, nt_core])
    wout_d = c.din("w_out", [D, D])
    on_d = c.din("o_norm", [512])
    fn_d = c.din("ffn_norm", [D])
    rt_d = c.din("router", [D, NEXP])
    id_d = c.din("ident", [128, 128])
    x1_d = c.dout("x1T", [D, nt_core])
    h2_d = c.dout("h2", [nt_core, D], BF16)
    aff_d = c.dout("aff", [nt_core, NEXP])

    ident, identb = load_const_p(c, id_d, [128, 128], "identc")
    og, ogb = load_col_vec(c, on_d, 512, "og")
    fg, fgb = load_col_vec(c, fn_d, D, "fg")
    ones = c.sb([128, 128], F32, "ones")
    onesb = Buf()
    P.memset(ones[:, :], 1.0, w=[onesb])
    Wo, Wob = load_w_bf16(c, wout_d, KC, D, "Wo")
    rt = c.sb([128, KC, NEXP], F32, "rt")
    rtb = Buf()
    P.dma(rt[:, :, :], rt_d.rearrange("(c p) e -> p c e", p=128), w=[rtb])
    banks = [c.ps([128, 512], F32, f"c_bank{i}") for i in range(8)]
    bankb = [Buf(psum=True) for _ in range(8)]
    xT = c.sb([128, KC, tb], F32, "xT")
    xb = Buf()
    om = c.sb([128, 4, tb], F32, "om")
    omb = Buf()
    yrw = c.sb([128, 4, tb], F32, "yrw")
    yrwb = Buf()
    ycat = c.sb([128, KC, tb], BF16, "ycat")
    ycatb = Buf()
    sq = c.sb([128, KC, tb], F32, "sq")
    sqb = Buf()
    tmp = c.sb([128, tb], F32, "tmp")
    tmpb = Buf()
    x1 = c.sb([128, KC, tb], F32, "x1")
    x1b = Buf()
    h2T = c.sb([128, KC, tb], F32, "h2T")
    h2Tb = Buf()
    lg = c.sb([128, 4, NEXP], F32, "lg")
    lgb = Buf()
    mx = c.sb([128, 4], F32, "mx")
    mxb = Buf()
    h2tok = [c.sb([128, D], BF16, f"h2tok{i}") for i in range(2)]
    h2tokb = [Buf(), Buf()]
    nblk = nt_core // tb
    for blk in range(nblk):
        t0 = blk * tb
        tsl = slice(t0, t0 + tb)
        P.dma(xT[:, :, :], xT_d[:, tsl].rearrange("(c p) t -> p c t", p=128), w=[xb])
        P.dma(om[:, :, :], om_d[:, tsl].rearrange("(c p) t -> p c t", p=128), w=[omb])
        P.dma(yrw[:, :, :], yrw_d[:, tsl].rearrange("(c p) t -> p c t", p=128), w=[yrwb])
        P.copy(ycat[:, 0:4, :], yrw[:, :, :], r=[yrwb], w=[ycatb], eng="gpsimd")
        fm_rmsnorm(c, om, omb, og, ogb, ycat[:, 4:8, :], ycatb, ones, onesb, banks[0], bankb[0],
                   tmp, tmpb, tb, kc=4, dim=512, sq=sq, sqb=sqb)
        for oc in range(KC):
            q = 1 + oc % 3
            for ch in range(KC):
                P.mm(banks[q][:, :], Wo[:, ch, oc * 128:(oc + 1) * 128], ycat[:, ch, :],
                     start=(ch == 0), stop=(ch == KC - 1), r=[Wob, ycatb], w=[bankb[q]])
            P.tt(x1[:, oc, :], banks[q][:, :], xT[:, oc, :], ALU.add, r=[bankb[q], xb], w=[x1b])
        P.dma(x1_d[:, tsl].rearrange("(c p) t -> p c t", p=128), x1[:, :, :], r=[x1b], is_out=True,
              eng="gpsimd")
        fm_rmsnorm(c, x1, x1b, fg, fgb, h2T, h2Tb, ones, onesb, banks[0], bankb[0], tmp, tmpb, tb,
                   sq=sq, sqb=sqb)
        for sub in range(4):
            for ch in range(KC):
                P.mm(banks[4][:, sub * NEXP:(sub + 1) * NEXP], h2T[:, ch, sub * 128:(sub + 1) * 128],
                     rt[:, ch, :], start=(ch == 0), stop=(ch == KC - 1), r=[h2Tb, rtb],
                     w=[bankb[4]])
        lgv = banks[4][:, 0:4 * NEXP].rearrange("p (s e) -> p s e", e=NEXP)
        P.op("vector", lambda e, lgv=lgv: e.reduce_max(out=mx[:, :], in_=lgv, axis=AX.X),
             r=[bankb[4]], w=[mxb])
        P.tt(lg[:, :, :], lgv, mx[:, :].unsqueeze(2).to_broadcast([128, 4, NEXP]), ALU.subtract,
             r=[bankb[4], mxb], w=[lgb])
        P.act(lg[:, :, :], lg[:, :, :], AF.Exp, r=[lgb], w=[lgb])
        P.op("vector", lambda e: e.reduce_sum(out=mx[:, :], in_=lg[:, :, :], axis=AX.X), r=[lgb],
             w=[mxb])
        P.op("vector", lambda e: e.reciprocal(out=mx[:, :], in_=mx[:, :]), r=[mxb], w=[mxb])
        P.tt(lg[:, :, :], lg[:, :, :], mx[:, :].unsqueeze(2).to_broadcast([128, 4, NEXP]), ALU.mult,
             r=[lgb, mxb], w=[lgb])
        P.dma(aff_d[tsl, :].rearrange("(s p) e -> p s e", p=128), lg[:, :, :], r=[lgb], is_out=True,
              eng="gpsimd")
        for sub in range(4):
            s2 = sub % 2
            for ch in range(KC):
                q = 5 + ch // 4
                P.tr(banks[q][:, (ch % 4) * 128:(ch % 4 + 1) * 128], h2T[:, ch, sub * 128:(sub + 1) * 128],
                     ident[:, :], r=[h2Tb, identb], w=[bankb[q]])
            P.copy(h2tok[s2][:, 0:512], banks[5][:, :], r=[bankb[5]], w=[h2tokb[s2]], eng="scalar")
            P.copy(h2tok[s2][:, 512:1024], banks[6][:, :], r=[bankb[6]], w=[h2tokb[s2]])
            P.dma(h2_d[t0 + sub * 128:t0 + (sub + 1) * 128, :], h2tok[s2][:, :], r=[h2tokb[s2]],
                  is_out=True, eng="gpsimd")
    return c.finish()


def phase_d_consts(T):
    idx = np.arange(128)
    tris = (idx[:, None] < idx[None, :]).astype(np.float32)
    nj = T // 128
    j = np.arange(nj)
    trij = (j[:, None] < j[None, :]).astype(np.float32)
    iota = np.tile(np.arange(CAP, dtype=np.float32)[None, :], (128, 1))
    return {"tris": tris, "trij": trij, "iota": iota, "ones": np.ones((128, 128), np.float32),
            "ident": np.eye(128, dtype=np.float32)}


def build_phase_d(T, NB, n_iter=34):
    c = Ctx()
    P = c.P
    NJ = T // 128
    NPR = 2 * NB
    aff_d = c.din("affT", [2, NB, T])
    h2_d = c.din("h2", [NB, T, D], BF16)
    wg_d = c.din("w_gate", [2, D, D])
    wu_d = c.din("w_up", [2, D, D])
    wd_d = c.din("w_down", [2, D, D])
    tris_d = c.din("tris", [128, 128])
    trij_d = c.din("trij", [NJ, NJ])
    iota_d = c.din("iota", [128, CAP])
    ones_d = c.din("ones", [128, 128])
    ye_d = c.dout("ye", [2, NB, CAP, D], BF16)
    posm_d = c.dout("posm", [2, NB, T])

    tris, trisb = load_const_p(c, tris_d, [128, 128], "tris")
    trij, trijb = load_const_p(c, trij_d, [NJ, NJ], "trij")
    iota, iotab = load_const_p(c, iota_d, [128, CAP], "iota")
    ones, onesb = load_const_p(c, ones_d, [128, 128], "onesd")
    banks = [c.ps([128, 512], F32, f"d_bank{i}") for i in range(8)]
    bankb = [Buf(psum=True) for _ in range(8)]
    afft = c.sb([128, NPR, NJ], F32, "afft")
    afftb = Buf()
    for e in range(2):
        for b in range(NB):
            P.dma(afft[:, e * NB + b, :], aff_d[e, b, :].rearrange("(j p) -> p j", p=128), w=[afftb],
                  allow_slow_non_contiguous=True)
    lo = c.sb([128, NPR], F32, "lo")
    hi = c.sb([128, NPR], F32, "hi")
    mid = c.sb([128, NPR], F32, "mid")
    cnt = c.sb([128, NPR], F32, "cnt")
    ge = c.sb([128, NPR], F32, "ge")
    dl = c.sb([128, NPR], F32, "dl")
    cmpj = c.sb([128, NPR, NJ], F32, "cmpj")
    B_ = {k: Buf() for k in ("lo", "hi", "mid", "cnt", "ge", "dl", "cmp")}
    P.memset(lo[:, :], 0.0, w=[B_["lo"]])
    P.memset(hi[:, :], 1.0001, w=[B_["hi"]])
    P.memset(mid[:, :], 0.5, w=[B_["mid"]])
    for it in range(n_iter):
        P.tt(cmpj[:, :, :], afft[:, :, :], mid[:, :].unsqueeze(2).to_broadcast([128, NPR, NJ]),
             ALU.is_ge, r=[afftb, B_["mid"]], w=[B_["cmp"]])
        P.op("vector", lambda e: e.reduce_sum(out=cnt[:, :], in_=cmpj[:, :, :], axis=AX.X),
             r=[B_["cmp"]], w=[B_["cnt"]])
        P.mm(banks[0][:, 0:NPR], ones[:, :], cnt[:, :], r=[onesb, B_["cnt"]], w=[bankb[0]])
        P.ts(ge[:, :], banks[0][:, 0:NPR], float(CAP), ALU.is_ge, r=[bankb[0]], w=[B_["ge"]])
        P.tt(dl[:, :], mid[:, :], lo[:, :], ALU.subtract, r=[B_["mid"], B_["lo"]], w=[B_["dl"]])
        P.tt(dl[:, :], dl[:, :], ge[:, :], ALU.mult, r=[B_["dl"], B_["ge"]], w=[B_["dl"]])
        P.tt(lo[:, :], lo[:, :], dl[:, :], ALU.add, r=[B_["lo"], B_["dl"]], w=[B_["lo"]])
        P.tt(dl[:, :], hi[:, :], mid[:, :], ALU.subtract, r=[B_["hi"], B_["mid"]], w=[B_["dl"]])
        P.tt(dl[:, :], dl[:, :], ge[:, :], ALU.mult, r=[B_["dl"], B_["ge"]], w=[B_["dl"]])
        P.tt(hi[:, :], mid[:, :], dl[:, :], ALU.add, r=[B_["mid"], B_["dl"]], w=[B_["hi"]])
        P.tt(mid[:, :], lo[:, :], hi[:, :], ALU.add, r=[B_["lo"], B_["hi"]], w=[B_["mid"]])
        P.ts(mid[:, :], mid[:, :], 0.5, ALU.mult, r=[B_["mid"]], w=[B_["mid"]])
    mask = c.sb([128, NPR, NJ], F32, "mask")
    maskb = Buf()
    P.tt(mask[:, :, :], afft[:, :, :], lo[:, :].unsqueeze(2).to_broadcast([128, NPR, NJ]),
         ALU.is_ge, r=[afftb, B_["lo"]], w=[maskb])
    totT = c.sb([NJ, NPR, 128], F32, "totT")
    totTb = Buf()
    pos = c.sb([128, NPR, NJ], F32, "pos")
    posb = Buf()
    posm = c.sb([128, NPR, NJ], F32, "posm")
    posmb = Buf()
    for pr in range(NPR):
        P.mm(banks[1][0:NJ, 0:128], mask[:, pr, :], ones[:, :], r=[maskb, onesb], w=[bankb[1]])
        P.copy(totT[:, pr, :], banks[1][0:NJ, 0:128], r=[bankb[1]], w=[totTb])
        P.mm(banks[2][:, 0:NJ], tris[:, :], mask[:, pr, :], start=True, stop=False,
             r=[trisb, maskb], w=[bankb[2]])
        P.mm(banks[2][:, 0:NJ], totT[:, pr, :], trij[:, :], start=False, stop=True,
             r=[totTb, trijb], w=[bankb[2]])
        P.copy(pos[:, pr, :], banks[2][:, 0:NJ], r=[bankb[2]], w=[posb])
    P.stt(posm[:, :, :], pos[:, :, :], 1.0, mask[:, :, :], ALU.add, ALU.mult, r=[posb, maskb],
          w=[posmb])
    P.ts(posm[:, :, :], posm[:, :, :], -1.0, ALU.add, r=[posmb], w=[posmb])
    for e in range(2):
        for b in range(NB):
            P.dma(posm_d[e, b, :].rearrange("(j p) -> p j", p=128), posm[:, e * NB + b, :],
                  r=[posmb], is_out=True, eng="gpsimd", allow_slow_non_contiguous=True)
    OH = c.sb([128, NJ, 512], BF16, "OH")
    OHb = Buf()
    h2t = [c.sb([128, D], BF16, f"h2t{i}") for i in range(3)]
    h2tb = [Buf() for _ in range(3)]
    xeT = c.sb([128, KC, CAP], BF16, "xeT")
    xeTb = Buf()
    hidT = c.sb([128, KC, CAP], BF16, "hidT")
    hidTb = Buf()
    sg = [c.sb([128, 512], F32, f"sg{i}") for i in range(2)]
    sgb = [Buf(), Buf()]
    yo = [c.sb([128, D], BF16, f"yo{i}") for i in range(2)]
    yob = [Buf(), Buf()]
    Wg = c.sb([128, KC, D], BF16, "Wg")
    Wu = c.sb([128, KC, D], BF16, "Wu")
    Wd = c.sb([128, KC, D], BF16, "Wd")
    Wgb, Wub, Wdb = Buf(), Buf(), Buf()
    stg = [c.sb([128, D], F32, f"wstg{i}") for i in range(2)]
    stgb = [Buf(), Buf()]
    sk = 0
    for e in range(2):
        for (W, Wb, wd_) in ((Wg, Wgb, wg_d), (Wu, Wub, wu_d), (Wd, Wdb, wd_d)):
            for ch in range(KC):
                s = sk % 2
                sk += 1
                P.dma(stg[s][:, :], wd_[e, ch * 128:(ch + 1) * 128, :], w=[stgb[s]])
                P.copy(W[:, ch, :], stg[s][:, :], r=[stgb[s]], w=[Wb], eng="gpsimd")
        for b in range(NB):
            pr = e * NB + b
            for half in range(2):
                for j in range(NJ):
                    P.ts(OH[:, j, :], iota[:, half * 512:(half + 1) * 512], posm[:, pr, j:j + 1],
                         ALU.is_equal, r=[iotab, posmb], w=[OHb])
                for j in range(NJ):
                    s3 = j % 3
                    P.dma(h2t[s3][:, :], h2_d[b, j * 128:(j + 1) * 128, :], w=[h2tb[s3]])
                    for ch in range(KC):
                        P.mm(banks[ch][:, :], h2t[s3][:, ch * 128:(ch + 1) * 128], OH[:, j, :],
                             start=(j == 0), stop=(j == NJ - 1), r=[h2tb[s3], OHb], w=[bankb[ch]])
                for ch in range(KC):
                    P.copy(xeT[:, ch, half * 512:(half + 1) * 512], banks[ch][:, :], r=[bankb[ch]],
                           w=[xeTb], eng=("scalar" if ch % 2 else "vector"))
            k = 0
            for fc in range(KC):
                for half in range(2):
                    hs = slice(half * 512, (half + 1) * 512)
                    qg, qu = 2 * (k % 4), 2 * (k % 4) + 1
                    s2 = k % 2
                    k += 1
                    for ch in range(KC):
                        P.mm(banks[qg][:, :], Wg[:, ch, fc * 128:(fc + 1) * 128], xeT[:, ch, hs],
                             start=(ch == 0), stop=(ch == KC - 1), r=[Wgb, xeTb], w=[bankb[qg]])
                    for ch in range(KC):
                        P.mm(banks[qu][:, :], Wu[:, ch, fc * 128:(fc + 1) * 128], xeT[:, ch, hs],
                             start=(ch == 0), stop=(ch == KC - 1), r=[Wub, xeTb], w=[bankb[qu]])
                    P.act(sg[s2][:, :], banks[qg][:, :], AF.Silu, r=[bankb[qg]], w=[sgb[s2]])
                    P.tt(hidT[:, fc, hs], banks[qu][:, :], sg[s2][:, :], ALU.mult,
                         r=[bankb[qu], sgb[s2]], w=[hidTb])
            for sbk in range(CAP // 128):
                s2 = sbk % 2
                for half in range(2):
                    q = (2 * sbk + half) % 8
                    for fc in range(KC):
                        P.mm(banks[q][:, :], hidT[:, fc, sbk * 128:(sbk + 1) * 128],
                             Wd[:, fc, half * 512:(half + 1) * 512], start=(fc == 0),
                             stop=(fc == KC - 1), r=[hidTb, Wdb], w=[bankb[q]])
                    P.copy(yo[s2][:, half * 512:(half + 1) * 512], banks[q][:, :], r=[bankb[q]],
                           w=[yob[s2]], eng=("scalar" if half else "vector"))
                P.dma(ye_d[e, b, sbk * 128:(sbk + 1) * 128, :], yo[s2][:, :], r=[yob[s2]],
                      is_out=True, eng="gpsimd")
    return c.finish()


def build_phase_e(nt_core, final, tb=512):
    c = Ctx()
    P = c.P
    x1_d = c.din("x1T", [D, nt_core])
    ye_d = c.din("ye", [NEXP, CAP, D], BF16)
    posm_d = c.din("posm", [NEXP, nt_core])
    aff_d = c.din("affT", [NEXP, nt_core])
    pT_d = c.din("pT", [256, nt_core])
    pn_d = c.din("ple_norm", [D])
    pp_d = c.din("ple_proj", [256, D])
    pg_d = c.din("ple_gate", [D, D])
    sid_d = c.din("slotid", [128, 8])
    fn_d = c.din("final_norm", [D])
    out_d = c.dout("x3T", [D, nt_core])

    sid, sidb = load_const_p(c, sid_d, [128, 8], "sid")
    pg, pgb = load_col_vec(c, pn_d, D, "pn")
    fg, fgb = load_col_vec(c, fn_d, D, "fn")
    ones = c.sb([128, 128], F32, "ones")
    onesb = Buf()
    P.memset(ones[:, :], 1.0, w=[onesb])
    Wgt, Wgtb = load_w_bf16(c, pg_d, KC, D, "Wpg")
    Wp, Wpb = load_w_bf16(c, pp_d, 2, D, "Wpp")
    banks = [c.ps([128, 512], F32, f"e_bank{i}") for i in range(8)]
    bankb = [Buf(psum=True) for _ in range(8)]
    x1 = c.sb([128, KC, tb], F32, "x1")
    x1b = Buf()
    x2 = c.sb([128, KC, tb], F32, "x2")
    x2b = Buf()
    h3 = c.sb([128, KC, tb], BF16, "h3")
    h3b = Buf()
    sq = c.sb([128, KC, tb], F32, "sq")
    sqb = Buf()
    tmp = c.sb([128, tb], F32, "tmp")
    tmpb = Buf()
    pT = c.sb([128, 2, tb], F32, "pT")
    pTb = Buf()
    pTh = c.sb([128, 2, tb], BF16, "pTh")
    pThb = Buf()
    posr = [c.sb([128, tb], F32, f"posr{i}") for i in range(2)]
    affr = [c.sb([128, tb], F32, f"affr{i}") for i in range(2)]
    rowb = [Buf(), Buf()]
    OHT = [c.sb([128, 8, tb], BF16, f"OHT{i}") for i in range(2)]
    OHTb = [Buf(), Buf()]
    yet = [c.sb([128, 8, D], BF16, f"yet{i}") for i in range(2)]
    yetb = [Buf(), Buf()]
    sgm = c.sb([128, tb], F32, "sgm")
    sgmb = Buf()
    nblk = nt_core // tb
    for blk in range(nblk):
        t0 = blk * tb
        tsl = slice(t0, t0 + tb)
        P.dma(x1[:, :, :], x1_d[:, tsl].rearrange("(c p) t -> p c t", p=128), w=[x1b])
        P.dma(pT[:, :, :], pT_d[:, tsl].rearrange("(c p) t -> p c t", p=128), w=[pTb])
        for e in range(NEXP):
            s = e % 2
            P.dma(posr[s][:, :], posm_d[e, tsl].partition_broadcast(128), w=[rowb[s]])
            P.dma(affr[s][:, :], aff_d[e, tsl].partition_broadcast(128), w=[rowb[s]])
            P.dma(yet[s][:, :, :], ye_d[e, :, :].rearrange("(sb p) d -> p sb d", p=128),
                  w=[yetb[s]])
            for sbk in range(8):
                P.stt(OHT[s][:, sbk, :], posr[s][:, :], sid[:, sbk:sbk + 1], affr[s][:, :],
                      ALU.is_equal, ALU.mult, r=[rowb[s], sidb], w=[OHTb[s]])
            for dc in range(KC):
                for sbk in range(8):
                    P.mm(banks[dc][:, :], yet[s][:, sbk, dc * 128:(dc + 1) * 128], OHT[s][:, sbk, :],
                         start=(e == 0 and sbk == 0), stop=(e == NEXP - 1 and sbk == 7),
                         r=[yetb[s], OHTb[s]], w=[bankb[dc]])
        for dc in range(KC):
            P.tt(x2[:, dc, :], banks[dc][:, :], x1[:, dc, :], ALU.add, r=[bankb[dc], x1b], w=[x2b])
        fm_rmsnorm(c, x2, x2b, pg, pgb, h3, h3b, ones, onesb, banks[0], bankb[0], tmp, tmpb, tb,
                   sq=sq, sqb=sqb)
        P.copy(pTh[:, :, :], pT[:, :, :], r=[pTb], w=[pThb], eng="gpsimd")
        for oc in range(KC):
            qa, qb = 1 + 2 * (oc % 3), 2 + 2 * (oc % 3)
            for ch in range(KC):
                P.mm(banks[qa][:, :], Wgt[:, ch, oc * 128:(oc + 1) * 128], h3[:, ch, :],
                     start=(ch == 0), stop=(ch == KC - 1), r=[Wgtb, h3b], w=[bankb[qa]])
            for ch in range(2):
                P.mm(banks[qb][:, :], Wp[:, ch, oc * 128:(oc + 1) * 128], pTh[:, ch, :],
                     start=(ch == 0), stop=(ch == 1), r=[Wpb, pThb], w=[bankb[qb]])
            P.act(sgm[:, :], banks[qa][:, :], AF.Sigmoid, r=[bankb[qa]], w=[sgmb])
            P.tt(sgm[:, :], banks[qb][:, :], sgm[:, :], ALU.mult, r=[bankb[qb], sgmb], w=[sgmb])
            P.tt(x2[:, oc, :], x2[:, oc, :], sgm[:, :], ALU.add, r=[x2b, sgmb], w=[x2b],
                 eng="gpsimd")
        if final:
            fm_rmsnorm(c, x2, x2b, fg, fgb, x1, x1b, ones, onesb, banks[0], bankb[0], tmp, tmpb, tb,
                       sq=sq, sqb=sqb)
            P.dma(out_d[:, tsl].rearrange("(c p) t -> p c t", p=128), x1[:, :, :], r=[x1b],
                  is_out=True, eng="gpsimd")
        else:
            P.dma(out_d[:, tsl].rearrange("(c p) t -> p c t", p=128), x2[:, :, :], r=[x2b],
                  is_out=True, eng="gpsimd")
    return c.finish()


_NC_CACHE = {}
_DBG = None


def _get(key, fn):
    if key not in _NC_CACHE:
        _NC_CACHE[key] = fn()
    return _NC_CACHE[key]


def _c(a):
    return np.ascontiguousarray(a)


def kernel(**inputs):
    inp = {k: np.asarray(v) for k, v in inputs.items()}
    x = inp["x"]
    B_, T_, D_ = x.shape
    L_ = inp["w_in"].shape[0]
    NTOK = B_ * T_
    NTC = NTOK // NCORES
    CPB = NCORES // B_
    xT_c = [_c(x.reshape(NTOK, D_)[i * NTC:(i + 1) * NTC].T) for i in range(NCORES)]
    mc = mla_consts()
    dc = phase_d_consts(T_)
    slotid = (np.arange(8, dtype=np.float32)[None, :] * 128 + np.arange(128, dtype=np.float32)[:, None])
    slotid = _c(slotid.astype(np.float32))
    pos = inp["positions"].astype(np.int32)
    for l in range(L_):
        ncA = _get(("A", NTC), lambda: build_phase_a(NTC))
        res = run_spmd(ncA, [{"xT": xT_c[i], "w_in": _c(inp["w_in"][l]), "g": _c(inp["attn_norm"][l])}
                             for i in range(NCORES)])
        zT = np.concatenate([r["zT"] for r in res], axis=1)
        z = zT.T.reshape(B_, T_, IN_COLS)
        if _DBG is not None:
            _DBG[f'z{l}'] = z.copy()
        zm = _c(z[:, :, 1920:].transpose(0, 2, 1))
        ncM = _get(("M", T_, B_), lambda: build_mla(T_, B_))
        q_up = inp["mla_q_up"][l]
        kv_up = inp["mla_kv_up"][l]
        maps = []
        for h in range(8):
            m = {"zm": zm, "pos": pos, "q_norm": _c(inp["mla_q_norm"][l]),
                 "kv_norm": _c(inp["mla_kv_norm"][l]), "q_up": _c(q_up[:, h * 96:(h + 1) * 96]),
                 "kv_up_k": _c(kv_up[:, h * 128:h * 128 + 64]),
                 "kv_up_v": _c(kv_up[:, h * 128 + 64:(h + 1) * 128])}
            m.update(mc)
            maps.append(m)
        resM = run_spmd(ncM, maps)
        o_mlaT = np.concatenate([r["o"] for r in resM], axis=1)
        ncR = _get(("R", T_, B_), lambda: build_rwkv(T_, B_))
        params = [inp[k][l] for k in ["rw_mu", "rw_w0", "rw_w_up", "rw_a0", "rw_a_up", "rw_g_up",
                                      "rw_k_k", "rw_k_a", "rw_r_k", "rw_lnx_w", "rw_lnx_b"]]
        z_rw = z[:, :, :1920]
        resR = run_spmd(ncR, [rwkv_in_map(h, z_rw, params) for h in range(8)])
        y_rw = np.concatenate([r["y_rw"] for r in resR], axis=2)
        del z, zT, zm, z_rw
        if _DBG is not None:
            _DBG[f'yrw{l}'] = y_rw
            _DBG[f'omla{l}'] = o_mlaT.transpose(0, 2, 1)
        ncC = _get(("C", NTC), lambda: build_phase_c(NTC))
        yrwT = y_rw.reshape(NTOK, 512)
        maps = []
        for i in range(NCORES):
            sl = slice(i * NTC, (i + 1) * NTC)
            bi, ti = (i * NTC) // T_, (i * NTC) % T_
            maps.append({"xT": xT_c[i], "yrwT": _c(yrwT[sl].T), "omT": _c(o_mlaT[bi, :, ti:ti + NTC]),
                         "w_out": _c(inp["w_out"][l]), "o_norm": _c(inp["mla_o_norm"][l]),
                         "ffn_norm": _c(inp["ffn_norm"][l]), "router": _c(inp["router"][l]),
                         "ident": dc["ident"]})
        resC = run_spmd(ncC, maps)
        x1T_c = [r["x1T"] for r in resC]
        h2 = np.concatenate([r["h2"] for r in resC], axis=0).reshape(B_, T_, D_)
        aff = np.concatenate([r["aff"] for r in resC], axis=0)
        affT = _c(aff.reshape(B_, T_, NEXP).transpose(2, 0, 1))
        if _DBG is not None:
            _DBG[f'aff{l}'] = aff
            _DBG[f'x1T{l}'] = x1T_c
            _DBG[f'h2{l}'] = h2
        ncD = _get(("D", T_, B_), lambda: build_phase_d(T_, B_))
        maps = []
        for i in range(NCORES):
            es = slice(2 * i, 2 * i + 2)
            maps.append({"affT": _c(affT[es]), "h2": h2, "w_gate": _c(inp["exp_w_gate"][l][es]),
                         "w_up": _c(inp["exp_w_up"][l][es]), "w_down": _c(inp["exp_w_down"][l][es]),
                         "tris": dc["tris"], "trij": dc["trij"], "iota": dc["iota"],
                         "ones": dc["ones"]})
        resD = run_spmd(ncD, maps)
        ye = np.concatenate([r["ye"] for r in resD], axis=0)
        posm = np.concatenate([r["posm"] for r in resD], axis=0)
        if _DBG is not None:
            _DBG[f'posm{l}'] = posm
            _DBG[f'ye{l}'] = ye
        final = (l == L_ - 1)
        ncE = _get(("E", NTC, final), lambda: build_phase_e(NTC, final))
        maps = []
        for i in range(NCORES):
            b = i // CPB
            tsl = slice((i % CPB) * NTC, (i % CPB + 1) * NTC)
            maps.append({"x1T": x1T_c[i], "ye": _c(ye[:, b]), "posm": _c(posm[:, b, tsl]),
                         "affT": _c(affT[:, b, tsl]), "pT": _c(inp["p"][l, b, tsl].T),
                         "ple_norm": _c(inp["ple_norm"][l]), "ple_proj": _c(inp["ple_proj"][l]),
                         "ple_gate": _c(inp["ple_gate"][l]), "slotid": slotid,
                         "final_norm": _c(inp["final_norm"])})
        resE = run_spmd(ncE, maps)
        xT_c = [r["x3T"] for r in resE]
    out = np.concatenate([t.T for t in xT_c], axis=0).reshape(B_, T_, D_)
    return np.ascontiguousarray(out.astype(np.float32))
```

```python
import numpy as np
from contextlib import ExitStack
import concourse.bass as bass
import concourse.mybir as mybir
from concourse.bass_utils import run_bass_kernel_spmd

F32 = mybir.dt.float32
BF16 = mybir.dt.bfloat16
I32 = mybir.dt.int32
AF = mybir.ActivationFunctionType
ALU = mybir.AluOpType
AX = mybir.AxisListType

NCORES = 8
SEM_EPOCH = 30000
NSLOT = 16


class Buf:
    __slots__ = ("last_w", "readers", "name", "psum")

    def __init__(self, name="", psum=False):
        self.last_w = None
        self.readers = []
        self.name = name
        self.psum = psum


class Op:
    __slots__ = ("eng", "emit", "deps", "is_dma", "slot", "slot_val", "cnt", "needed", "idx")


class Prog:
    ENGS = ["tensor", "vector", "scalar", "gpsimd", "sync"]

    def __init__(self, nc):
        self.nc = nc
        self.ops = {e: [] for e in self.ENGS}
        self.slot_count = {}
        self.all_ops = []
        self.out_dmas = []

    def op(self, eng, emit, r=(), w=(), dma=False, is_out=False):
        o = Op()
        o.eng = eng
        o.emit = emit
        o.is_dma = dma
        o.needed = False
        o.cnt = 0
        o.idx = len(self.all_ops)
        deps = {}
        for b in r:
            if b.last_w is not None:
                deps[b.last_w.idx] = b.last_w
            if b.psum:
                for rd in b.readers:
                    if rd.eng != eng:
                        deps[rd.idx] = rd
        for b in w:
            if b.last_w is not None:
                deps[b.last_w.idx] = b.last_w
            for rd in b.readers:
                deps[rd.idx] = rd
        for b in r:
            b.readers.append(o)
        for b in w:
            b.last_w = o
            b.readers = []
        deps.pop(o.idx, None)
        if eng == "tensor":
            o.deps = [d for d in deps.values() if d.eng != "tensor"]
        else:
            o.deps = list(deps.values())
        if dma:
            key = eng
            n = self.slot_count.get(key, 0)
            self.slot_count[key] = n + 1
            o.slot = (key, n % NSLOT)
            o.slot_val = 16 * (n // NSLOT + 1)
        self.ops[eng].append(o)
        self.all_ops.append(o)
        if is_out:
            self.out_dmas.append(o)
        return o

    def dma(self, out, in_, r=(), w=(), eng="sync", is_out=False, **kw):
        return self.op(eng, lambda e: e.dma_start(out=out, in_=in_, **kw), r=r, w=w,
                       dma=True, is_out=is_out)

    def mm(self, out, lhsT, rhs, start=True, stop=True, r=(), w=(), nochk=False):
        if nochk:
            return self.op("tensor", lambda e: e.matmul(out, lhsT, rhs, start=start, stop=stop,
                                                        skip_group_check=True), r=r, w=w)
        return self.op("tensor", lambda e: e.matmul(out, lhsT, rhs, start=start, stop=stop),
                       r=r, w=w)

    def tr(self, out, in_, ident, r=(), w=()):
        return self.op("tensor", lambda e: e.transpose(out, in_, ident), r=r, w=w)

    def act(self, out, in_, func, r=(), w=(), **kw):
        return self.op("scalar", lambda e: e.activation(out=out, in_=in_, func=func, **kw),
                       r=r, w=w)

    def tt(self, out, in0, in1, op, r=(), w=(), eng="vector"):
        return self.op(eng, lambda e: e.tensor_tensor(out=out, in0=in0, in1=in1, op=op),
                       r=r, w=w)

    def ts(self, out, in0, s1, op0, s2=None, op1=None, r=(), w=(), eng="vector", **kw):
        if op1 is None:
            return self.op(eng, lambda e: e.tensor_scalar(out=out, in0=in0, scalar1=s1,
                                                          scalar2=None, op0=op0, **kw), r=r, w=w)
        return self.op(eng, lambda e: e.tensor_scalar(out=out, in0=in0, scalar1=s1, scalar2=s2,
                                                      op0=op0, op1=op1, **kw), r=r, w=w)

    def stt(self, out, in0, scalar, in1, op0, op1, r=(), w=(), eng="vector"):
        return self.op(eng, lambda e: e.scalar_tensor_tensor(out=out, in0=in0, scalar=scalar,
                                                             in1=in1, op0=op0, op1=op1), r=r, w=w)

    def copy(self, out, in_, r=(), w=(), eng="vector"):
        if eng == "scalar":
            return self.op(eng, lambda e: e.copy(out=out, in_=in_), r=r, w=w)
        return self.op(eng, lambda e: e.tensor_copy(out=out, in_=in_), r=r, w=w)

    def memset(self, ap, val, w=(), eng="vector"):
        return self.op(eng, lambda e: e.memset(ap, val), w=w)

    def emit(self, stack):
        nc = self.nc
        for o in self.all_ops:
            for d in o.deps:
                d.needed = True
        sems = {}
        for e in self.ENGS:
            if e == "sync":
                continue
            c = 0
            for o in self.ops[e]:
                if o.is_dma:
                    continue
                if o.needed:
                    c += 1
                    o.cnt = c
            nsem = (c + SEM_EPOCH - 1) // SEM_EPOCH
            sems[e] = [stack.enter_context(nc.semaphore(f"s_{e}_{i}")) for i in range(max(nsem, 1))]
        slot_sems = {}
        for key, n in self.slot_count.items():
            for s in range(min(n, NSLOT)):
                slot_sems[(key, s)] = stack.enter_context(nc.semaphore(f"d_{key}_{s}"))
        block = stack.enter_context(nc.Block())
        out_dmas = self.out_dmas

        def run(engname, eng):
            seen = {}

            def wait_for(d):
                if d.is_dma:
                    k = ("dma",) + d.slot
                    if seen.get(k, 0) >= d.slot_val:
                        return
                    seen[k] = d.slot_val
                    eng.wait_ge(slot_sems[d.slot], d.slot_val)
                else:
                    ep = (d.cnt - 1) // SEM_EPOCH
                    v = d.cnt - ep * SEM_EPOCH
                    k = (d.eng, ep)
                    if seen.get(k, 0) >= v:
                        return
                    seen[k] = v
                    eng.wait_ge(sems[d.eng][ep], v)

            for o in self.ops[engname]:
                for d in o.deps:
                    wait_for(d)
                if o.is_dma:
                    if o.slot_val > 16:
                        k = ("dma",) + o.slot
                        if seen.get(k, 0) < o.slot_val - 16:
                            seen[k] = o.slot_val - 16
                            eng.wait_ge(slot_sems[o.slot], o.slot_val - 16)
                    ins = o.emit(eng)
                    ins.then_inc(slot_sems[o.slot], 16)
                else:
                    ins = o.emit(eng)
                    if o.needed:
                        ep = (o.cnt - 1) // SEM_EPOCH
                        ins.then_inc(sems[engname][ep], 1)
            if engname == "sync":
                for o in out_dmas:
                    eng.wait_ge(slot_sems[o.slot], o.slot_val)
                for key, n in self.slot_count.items():
                    for s in range(min(n, NSLOT)):
                        last = 16 * ((n - 1 - s) // NSLOT + 1)
                        eng.wait_ge(slot_sems[(key, s)], last)

        @block.tensor
        def _(e):
            run("tensor", e)

        @block.vector
        def _(e):
            run("vector", e)

        @block.scalar
        def _(e):
            run("scalar", e)

        @block.gpsimd
        def _(e):
            run("gpsimd", e)

        @block.sync
        def _(e):
            run("sync", e)


class Ctx:
    def __init__(self):
        self.nc = bass.Bass("TRN2", target_bir_lowering=False)
        self.stack = ExitStack()
        self.P = Prog(self.nc)
        self.n = 0

    def sb(self, shape, dt=F32, name=None):
        self.n += 1
        return self.stack.enter_context(
            self.nc.sbuf_tensor(f"sb{self.n}_{name or ''}", list(shape), dt))

    def ps(self, shape, dt=F32, name=None):
        self.n += 1
        return self.stack.enter_context(
            self.nc.psum_tensor(f"ps{self.n}_{name or ''}", list(shape), dt))

    def din(self, name, shape, dt=F32):
        return self.nc.dram_tensor(name, list(shape), dt, kind="ExternalInput").ap()

    def dout(self, name, shape, dt=F32):
        return self.nc.dram_tensor(name, list(shape), dt, kind="ExternalOutput").ap()

    def finish(self):
        self.P.emit(self.stack)
        self.stack.close()
        return self.nc


def run_spmd(nc, in_maps):
    res = run_bass_kernel_spmd(nc, in_maps, core_ids=list(range(len(in_maps))))
    return res.results


D = 1024
KC = D // 128
EPS = 1e-6


def load_col_vec(c, vec_ap, n, name):
    P = c.P
    t = c.sb([128, n // 128], F32, name)
    b = Buf(name)
    P.dma(t[:, :], vec_ap.rearrange("(c p) -> p c", p=128), w=[b], allow_slow_non_contiguous=True)
    return t, b


def fm_rmsnorm(c, xT, xb, g, gb, hT, hb, ones, onesb, ps, psb, tmp, tmpb, nt, kc=KC, dim=D,
               sq=None, sqb=None):
    P = c.P
    for ch in range(kc):
        P.act(sq[:, ch, :nt], xT[:, ch, :nt], AF.Square, r=[xb], w=[sqb])
    for ch in range(kc):
        P.mm(ps[:, :nt], ones[:, :], sq[:, ch, :nt], start=(ch == 0), stop=(ch == kc - 1),
             r=[sqb, onesb], w=[psb])
    P.ts(tmp[:, :nt], ps[:, :nt], 1.0 / dim, ALU.mult, EPS, ALU.add, r=[psb], w=[tmpb])
    P.act(tmp[:, :nt], tmp[:, :nt], AF.Sqrt, r=[tmpb], w=[tmpb])
    P.op("vector", lambda e: e.reciprocal(out=tmp[:, :nt], in_=tmp[:, :nt]), r=[tmpb], w=[tmpb])
    for ch in range(kc):
        P.stt(hT[:, ch, :nt], xT[:, ch, :nt], g[:, ch:ch + 1], tmp[:, :nt], ALU.mult, ALU.mult,
              r=[xb, gb, tmpb], w=[hb])


IN_COLS = 2336


def build_phase_a(nt_core, tb=512):
    c = Ctx()
    P = c.P
    xT_d = c.din("xT", [D, nt_core])
    w_d = c.din("w_in", [D, IN_COLS])
    g_d = c.din("g", [D])
    zT_d = c.dout("zT", [IN_COLS, nt_core])

    g, gb = load_col_vec(c, g_d, D, "g_sb")
    ones = c.sb([128, 128], F32, "ones")
    onesb = Buf()
    P.memset(ones[:, :], 1.0, w=[onesb])
    W = c.sb([128, KC, IN_COLS], BF16, "W")
    Wb = Buf()
    stg = [c.sb([128, IN_COLS], F32, f"stg{i}") for i in range(2)]
    stgb = [Buf(), Buf()]
    for ch in range(KC):
        s = ch % 2
        P.dma(stg[s][:, :], w_d[ch * 128:(ch + 1) * 128, :], w=[stgb[s]])
        P.copy(W[:, ch, :], stg[s][:, :], r=[stgb[s]], w=[Wb], eng="gpsimd")
    xT = [c.sb([128, KC, tb], F32, f"xT{i}") for i in range(2)]
    xb = [Buf(), Buf()]
    sq = c.sb([128, KC, tb], F32, "sq")
    sqb = Buf()
    hT = [c.sb([128, KC, tb], BF16, f"hT{i}") for i in range(2)]
    hb = [Buf(), Buf()]
    tmp = c.sb([128, tb], F32, "tmp")
    tmpb = Buf()
    ps_ss = c.ps([128, tb], F32, "ps_ss")
    ps_ssb = Buf(psum=True)
    psz = [c.ps([128, tb], F32, f"psz{i}") for i in range(4)]
    pszb = [Buf(psum=True) for _ in range(4)]
    zo = [c.sb([128, tb], F32, f"zo{i}") for i in range(4)]
    zob = [Buf() for _ in range(4)]
    nblk = nt_core // tb
    ncb = (IN_COLS + 127) // 128
    k = 0
    for blk in range(nblk):
        s = blk % 2
        t0 = blk * tb
        P.dma(xT[s][:, :, :], xT_d[:, t0:t0 + tb].rearrange("(c p) t -> p c t", p=128), w=[xb[s]])
        fm_rmsnorm(c, xT[s], xb[s], g, gb, hT[s], hb[s], ones, onesb, ps_ss, ps_ssb, tmp, tmpb, tb,
                   sq=sq, sqb=sqb)
        for cb in range(ncb):
            c0 = cb * 128
            cw = min(128, IN_COLS - c0)
            q = k % 4
            k += 1
            for ch in range(KC):
                P.mm(psz[q][:cw, :], W[:, ch, c0:c0 + cw], hT[s][:, ch, :], start=(ch == 0),
                     stop=(ch == KC - 1), r=[Wb, hb[s]], w=[pszb[q]])
            P.copy(zo[q][:cw, :], psz[q][:cw, :], r=[pszb[q]], w=[zob[q]],
                   eng=("scalar" if cb % 2 else "vector"))
            P.dma(zT_d[c0:c0 + cw, t0:t0 + tb], zo[q][:cw, :], r=[zob[q]], is_out=True,
                  eng="gpsimd")
    return c.finish()


import math
QK_NOPE, QK_ROPE, V_HEAD, Q_LORA, KV_LORA = 64, 32, 64, 256, 128
HD = QK_NOPE + QK_ROPE
ROPE_THETA = 10000.0


def mla_consts():
    invf = np.zeros((128, 1), np.float32)
    f = (ROPE_THETA ** (-np.arange(0, QK_ROPE, 2, dtype=np.float32) / QK_ROPE)).astype(np.float32)
    invf[64:80, 0] = f
    invf[80:96, 0] = f
    pa = np.zeros((32, 96), np.float32)
    pb = np.zeros((32, 96), np.float32)
    for i in range(32):
        pa[i, 64 + i] = 1.0
    for i in range(16):
        pb[i, 80 + i] = 1.0
        pb[16 + i, 64 + i] = -1.0
    return {"invf": invf, "placeA": pa, "placeB": pb}


def emit_mla(c, T, NB, zm_d, pos_d, qn_d, kvn_d, qup_d, kvupk_d, kvupv_d, invf_d, pa_d, pb_d, o_d):
    P = c.P
    TB = 512
    scale = float(HD) ** -0.5
    ones = c.sb([128, 128], F32, "m_ones")
    onesb = Buf()
    P.memset(ones[:, :], 1.0, w=[onesb])
    invf = c.sb([128, 1], F32, "m_invf")
    invfb = Buf()
    P.dma(invf[:, :], invf_d[:, :], w=[invfb])
    plf = c.sb([32, 2, 96], F32, "m_plf")
    plfb = Buf()
    P.dma(plf[:, 0, :], pa_d[:, :], w=[plfb])
    P.dma(plf[:, 1, :], pb_d[:, :], w=[plfb])
    pl = c.sb([32, 2, 96], BF16, "m_pl")
    plb = Buf()
    P.copy(pl[:, :, :], plf[:, :, :], r=[plfb], w=[plb])
    qg, qgb = load_col_vec(c, qn_d, Q_LORA, "m_qg")
    kg, kgb = load_col_vec(c, kvn_d, KV_LORA, "m_kg")
    wq = c.sb([128, 2, 96], F32, "m_wq")
    wqb = Buf()
    P.dma(wq[:, :, :], qup_d.rearrange("(c p) m -> p c m", p=128), w=[wqb])
    A = c.sb([128, 2, 96], BF16, "m_A")
    Bm = c.sb([128, 2, 96], BF16, "m_B")
    Ab = Buf()
    Bb = Buf()
    for ch in range(2):
        P.ts(wq[:, ch, :], wq[:, ch, :], qg[:, ch:ch + 1], ALU.mult, r=[wqb, qgb], w=[wqb])
    P.copy(A[:, :, :], wq[:, :, :], r=[wqb], w=[Ab])
    P.memset(Bm[:, :, :], 0.0, w=[Bb])
    P.ts(Bm[:, :, 64:80], wq[:, :, 80:96], -1.0, ALU.mult, r=[wqb], w=[Bb])
    P.copy(Bm[:, :, 80:96], wq[:, :, 64:80], r=[wqb], w=[Bb])
    wk = c.sb([128, 128], F32, "m_wk")
    wkb = Buf()
    P.dma(wk[:, 0:64], kvupk_d[:, :], w=[wkb])
    P.dma(wk[:, 64:128], kvupv_d[:, :], w=[wkb])
    P.ts(wk[:, :], wk[:, :], kg[:, 0:1], ALU.mult, r=[wkb, kgb], w=[wkb])
    KU = c.sb([128, 96], BF16, "m_KU")
    VU = c.sb([128, 64], BF16, "m_VU")
    KUb = Buf()
    VUb = Buf()
    P.memset(KU[:, :], 0.0, w=[KUb])
    P.copy(KU[:, 0:64], wk[:, 0:64], r=[wkb], w=[KUb])
    P.copy(VU[:, :], wk[:, 64:128], r=[wkb], w=[VUb])
    cos2 = c.sb([128, TB], F32, "m_cos")
    sin2 = c.sb([128, TB], F32, "m_sin")
    cosb = Buf()
    sinb = Buf()
    posi = c.sb([128, TB], I32, "m_posi")
    posib = Buf()
    ang = c.sb([128, TB], F32, "m_ang")
    angb = Buf()
    frc = c.sb([128, TB], F32, "m_frc")
    frcb = Buf()
    QT = c.sb([96, T], BF16, "m_QT")
    KT = c.sb([96, T], BF16, "m_KT")
    NTL = T // 128
    Vt = c.sb([128, NTL, 65], BF16, "m_Vt")
    nblk = T // TB
    QTb = [Buf() for _ in range(nblk)]
    KTb = [Buf() for _ in range(nblk)]
    Vtb = [Buf() for _ in range(nblk)]
    qd = [c.sb([128, 2, TB], F32, f"m_qd{i}") for i in range(2)]
    kvd = [c.sb([128, TB], F32, f"m_kvd{i}") for i in range(2)]
    kr = [c.sb([32, TB], F32, f"m_kr{i}") for i in range(2)]
    inb = [Buf(), Buf()]
    sq = c.sb([128, 3, TB], F32, "m_sq")
    sqb = Buf()
    rs = c.sb([128, 2, TB], F32, "m_rs")
    rsb = Buf()
    qn = c.sb([128, 2, TB], BF16, "m_qn")
    kvn = c.sb([128, TB], BF16, "m_kvn")
    krb16 = c.sb([32, TB], BF16, "m_krb")
    nb_ = Buf()
    t1 = c.sb([128, TB], F32, "m_t1")
    t2 = c.sb([128, TB], F32, "m_t2")
    t1b = Buf()
    t2b = Buf()
    NSB = 4
    pt = [c.sb([128, TB], BF16, f"m_pt{i}") for i in range(NSB)]
    ptb = [Buf() for _ in range(NSB)]
    osb = [c.sb([128, 4, 64], F32, f"m_o{i}") for i in range(2)]
    osbb = [Buf(), Buf()]
    rcp = c.sb([128, 4, 1], F32, "m_rcp")
    rcpb = Buf()
    bank = [c.ps([128, 512], F32, f"m_bank{i}") for i in range(8)]
    bankb = [Buf(psum=True) for _ in range(8)]
    TWO_PI = 2.0 * math.pi

    for b in range(NB):
        for blk in range(nblk):
            s = blk % 2
            t0 = blk * TB
            ts_ = slice(t0, t0 + TB)
            P.dma(posi[:, :], pos_d[b, ts_].partition_broadcast(128), w=[posib])
            P.copy(ang[:, :], posi[:, :], r=[posib], w=[angb])
            P.ts(ang[:, :], ang[:, :], invf[:, 0:1], ALU.mult, r=[angb, invfb], w=[angb])
            for (dst, dstb, ph) in ((sin2, sinb, 0.5), (cos2, cosb, 0.75)):
                R_ = slice(64, 96)
                P.ts(dst[R_, :], ang[R_, :], 1.0 / TWO_PI, ALU.mult, ph, ALU.add, r=[angb], w=[dstb])
                P.copy(posi[R_, :], dst[R_, :], r=[dstb], w=[posib])
                P.copy(frc[R_, :], posi[R_, :], r=[posib], w=[frcb])
                P.tt(dst[R_, :], dst[R_, :], frc[R_, :], ALU.subtract, r=[dstb, frcb], w=[dstb])
                P.ts(frc[R_, :], dst[R_, :], 0.0, ALU.is_lt, r=[dstb], w=[frcb])
                P.tt(dst[R_, :], dst[R_, :], frc[R_, :], ALU.add, r=[dstb, frcb], w=[dstb])
                P.ts(dst[R_, :], dst[R_, :], TWO_PI, ALU.mult, -math.pi, ALU.add, r=[dstb], w=[dstb])
                P.ts(dst[R_, :], dst[R_, :], -math.pi, ALU.max, math.pi, ALU.min, r=[dstb], w=[dstb])
                P.act(dst[R_, :], dst[R_, :], AF.Sin, r=[dstb], w=[dstb])
            P.dma(qd[s][:, :, :], zm_d[b, 0:256, ts_].rearrange("(c p) t -> p c t", p=128),
                  w=[inb[s]])
            P.dma(kvd[s][:, :], zm_d[b, 256:384, ts_], w=[inb[s]])
            P.dma(kr[s][:, :], zm_d[b, 384:416, ts_], w=[inb[s]])
            for ch in range(2):
                P.act(sq[:, ch, :], qd[s][:, ch, :], AF.Square, r=[inb[s]], w=[sqb])
            P.act(sq[:, 2, :], kvd[s][:, :], AF.Square, r=[inb[s]], w=[sqb])
            P.mm(bank[0][:, :], ones[:, :], sq[:, 0, :], start=True, stop=False, r=[sqb, onesb],
                 w=[bankb[0]])
            P.mm(bank[0][:, :], ones[:, :], sq[:, 1, :], start=False, stop=True, r=[sqb, onesb],
                 w=[bankb[0]])
            P.mm(bank[1][:, :], ones[:, :], sq[:, 2, :], r=[sqb, onesb], w=[bankb[1]])
            P.ts(rs[:, 0, :], bank[0][:, :], 1.0 / Q_LORA, ALU.mult, EPS, ALU.add, r=[bankb[0]],
                 w=[rsb])
            P.ts(rs[:, 1, :], bank[1][:, :], 1.0 / KV_LORA, ALU.mult, EPS, ALU.add, r=[bankb[1]],
                 w=[rsb])
            P.act(rs[:, :, :], rs[:, :, :], AF.Sqrt, r=[rsb], w=[rsb])
            P.op("vector", lambda e: e.reciprocal(out=rs[:, :, :], in_=rs[:, :, :]), r=[rsb], w=[rsb])
            for ch in range(2):
                P.tt(qn[:, ch, :], qd[s][:, ch, :], rs[:, 0, :], ALU.mult, r=[inb[s], rsb], w=[nb_])
            P.tt(kvn[:, :], kvd[s][:, :], rs[:, 1, :], ALU.mult, r=[inb[s], rsb], w=[nb_])
            P.copy(krb16[:, :], kr[s][:, :], r=[inb[s]], w=[nb_], eng="gpsimd")
            for ch in range(2):
                P.mm(bank[2][:96, :], A[:, ch, :], qn[:, ch, :], start=(ch == 0), stop=(ch == 1),
                     r=[Ab, nb_], w=[bankb[2]])
            for ch in range(2):
                P.mm(bank[3][:96, :], Bm[:, ch, :], qn[:, ch, :], start=(ch == 0), stop=(ch == 1),
                     r=[Bb, nb_], w=[bankb[3]])
            P.mm(bank[4][:96, :], KU[:, :], kvn[:, :], start=True, stop=False, r=[KUb, nb_],
                 w=[bankb[4]])
            P.mm(bank[4][:96, :], pl[:, 0, :], krb16[:, :], start=False, stop=True, r=[plb, nb_],
                 w=[bankb[4]])
            P.mm(bank[5][:96, :], pl[:, 1, :], krb16[:, :], r=[plb, nb_], w=[bankb[5]])
            for j in range(4):
                P.mm(bank[6][:, j * 64:(j + 1) * 64], kvn[:, j * 128:(j + 1) * 128], VU[:, :],
                     r=[VUb, nb_], w=[bankb[6]])
            P.copy(QT[0:64, ts_], bank[2][0:64, :], r=[bankb[2]], w=[QTb[blk]], eng="scalar")
            P.tt(t1[64:96, :], bank[3][64:96, :], sin2[64:96, :], ALU.mult, r=[bankb[3], sinb],
                 w=[t1b])
            P.tt(t2[64:96, :], bank[2][64:96, :], cos2[64:96, :], ALU.mult, r=[bankb[2], cosb],
                 w=[t2b])
            P.tt(QT[64:96, ts_], t1[64:96, :], t2[64:96, :], ALU.add, r=[t1b, t2b], w=[QTb[blk]],
                 eng="gpsimd")
            P.copy(KT[0:64, ts_], bank[4][0:64, :], r=[bankb[4]], w=[KTb[blk]], eng="scalar")
            P.tt(t1[64:96, :], bank[5][64:96, :], sin2[64:96, :], ALU.mult, r=[bankb[5], sinb],
                 w=[t1b])
            P.tt(t2[64:96, :], bank[4][64:96, :], cos2[64:96, :], ALU.mult, r=[bankb[4], cosb],
                 w=[t2b])
            P.tt(KT[64:96, ts_], t1[64:96, :], t2[64:96, :], ALU.add, r=[t1b, t2b], w=[KTb[blk]],
                 eng="gpsimd")
            P.copy(Vt[:, blk * 4:(blk + 1) * 4, 0:64],
                   bank[6][:, 0:256].rearrange("p (j v) -> p j v", v=64), r=[bankb[6]],
                   w=[Vtb[blk]])
            P.memset(Vt[:, blk * 4:(blk + 1) * 4, 64:65], 1.0, w=[Vtb[blk]], eng="gpsimd")
        for qb in range(nblk):
            qs = slice(qb * TB, (qb + 1) * TB)
            accb = bankb[7]
            acc = bank[7]
            def s_exp(kt):
                s = kt % NSB
                kb = kt // 4
                P.mm(bank[s][:, :], KT[:, kt * 128:(kt + 1) * 128], QT[:, qs], r=[KTb[kb], QTb[qb]],
                     w=[bankb[s]])
                P.act(pt[s][:, :], bank[s][:, :], AF.Exp, scale=scale, r=[bankb[s]], w=[ptb[s]])

            LOOK = NSB - 1
            for kt in range(min(LOOK, NTL)):
                s_exp(kt)
            for kt in range(NTL):
                if kt + LOOK < NTL:
                    s_exp(kt + LOOK)
                s = kt % NSB
                kb = kt // 4
                for j in range(4):
                    P.mm(acc[:, j * 65:(j + 1) * 65], pt[s][:, j * 128:(j + 1) * 128], Vt[:, kt, :],
                         start=(kt == 0 and j == 0), stop=(kt == NTL - 1), r=[ptb[s], Vtb[kb]],
                         w=[accb], nochk=True)
            so = qb % 2
            accv = acc[:, 0:260].rearrange("p (j v) -> p j v", v=65)
            P.op("vector", lambda e, accv=accv: e.reciprocal(out=rcp[:, :, :], in_=accv[:, :, 64:65]),
                 r=[accb], w=[rcpb])
            P.tt(osb[so][:, :, :], accv[:, :, 0:64], rcp[:, :, :].to_broadcast([128, 4, 64]),
                 ALU.mult, r=[accb, rcpb], w=[osbb[so]])
            P.dma(o_d[b, qb * TB:(qb + 1) * TB, :].rearrange("(j p) v -> p j v", p=128),
                  osb[so][:, :, :], r=[osbb[so]], is_out=True, eng="gpsimd")


def build_mla(T, NB):
    c = Ctx()
    zm_d = c.din("zm", [NB, 416, T])
    pos_d = c.din("pos", [NB, T], I32)
    qn_d = c.din("q_norm", [Q_LORA])
    kvn_d = c.din("kv_norm", [KV_LORA])
    qup_d = c.din("q_up", [Q_LORA, 96])
    kk_d = c.din("kv_up_k", [KV_LORA, 64])
    kv_d = c.din("kv_up_v", [KV_LORA, 64])
    invf_d = c.din("invf", [128, 1])
    pa_d = c.din("placeA", [32, 96])
    pb_d = c.din("placeB", [32, 96])
    o_d = c.dout("o", [NB, T, 64])
    emit_mla(c, T, NB, zm_d, pos_d, qn_d, kvn_d, qup_d, kk_d, kv_d, invf_d, pa_d, pb_d, o_d)
    return c.finish()


RW_N = 64
LOGW_SCALE = -0.6065306597126334
GN_EPS = 64e-5
RWC = 576


def rwkv_consts():
    idx = np.arange(128)
    s = idx[:, None]
    t = idx[None, :]
    le = (s <= t).astype(np.float32)
    lt = (s < t).astype(np.float32)
    gt = (s > t).astype(np.float32)
    lem = (idx <= 63).astype(np.float32)[:, None]
    f = {}
    f["ME1"] = le - lem
    f["ME2"] = lt - lem
    msel = np.zeros((128, 2), np.float32)
    msel[:, 0] = lem[:, 0]
    msel[:, 1] = 1.0
    f["MSEL"] = msel
    f["ME4"] = gt
    f["MASK4"] = np.concatenate([le, lt, le, lt], axis=1)
    f["STRT"] = gt.copy()
    out = {}
    for d in range(2):
        for k, v in f.items():
            if d == 0:
                m = v
            else:
                m = v[::-1]
                if k == "MASK4":
                    m = np.concatenate([m[:, i * 128:(i + 1) * 128][:, ::-1] for i in range(4)], 1)
                elif k != "MSEL":
                    m = m[:, ::-1]
            out[f"{k}{d}"] = np.ascontiguousarray(m, dtype=np.float32)
    out["ident"] = np.eye(128, dtype=np.float32)
    return out


RW_CONST_SHAPES = {"ME1": [128, 128], "ME2": [128, 128], "MSEL": [128, 2], "ME4": [128, 128],
                   "MASK4": [128, 512], "STRT": [128, 128]}


def rwkv_host_params(h, mu, w0, w_up, a0, a_up, g_up, k_k, k_a, r_k, lnx_w, lnx_b):
    hs = slice(h * 64, (h + 1) * 64)
    o3 = 1536
    cols = np.concatenate([np.arange(h * 64, h * 64 + 64), 512 + np.arange(h * 64, h * 64 + 64),
                           1024 + np.arange(h * 64, h * 64 + 64),
                           o3 + np.arange(0, 64), o3 + 128 + np.arange(0, 64),
                           o3 + 64 + np.arange(0, 64), o3 + 128 + 64 + np.arange(0, 64),
                           o3 + 256 + np.arange(0, 128)])
    p = {"rw_cols": cols, "mu": np.ascontiguousarray(mu[cols])}
    for d in range(2):
        bd = np.zeros((128, 128), np.float32)
        bd[0:64, 0:64] = w_up[d][:, hs]
        bd[64:128, 64:128] = a_up[d][:, hs]
        p[f"up_bd{d}"] = bd
        p[f"bias{d}"] = np.concatenate([w0[d, hs], a0[d, hs]]).astype(np.float32)
    p["g_up"] = np.ascontiguousarray(g_up[:, hs])
    p["k_k"] = np.ascontiguousarray(k_k[hs])
    p["k_a"] = np.ascontiguousarray(k_a[hs])
    p["r_k"] = np.ascontiguousarray(r_k[h])
    p["lnx_w"] = np.ascontiguousarray(lnx_w[hs])
    p["lnx_b"] = np.ascontiguousarray(lnx_b[hs])
    return p


def emit_rwkv(c, T, NB, zr_d, pd, cd, y_d):
    P = c.P
    nc = c.nc
    NT = T // 128
    def load_const(name, shape):
        t = c.sb(shape, F32, "k_" + name)
        b = Buf()
        P.dma(t[:, :], cd[name][:, :], w=[b])
        return t, b
    ident, identb = load_const("ident", [128, 128])
    K = {}
    for d in range(2):
        for k, shp in RW_CONST_SHAPES.items():
            K[(k, d)] = load_const(f"{k}{d}", shp)

    def bcast(name, n, scale=None):
        t = c.sb([128, n], F32, "b_" + name)
        b = Buf()
        P.dma(t[:, :], pd[name].partition_broadcast(128), w=[b])
        if scale is not None:
            P.ts(t[:, :], t[:, :], scale, ALU.mult, r=[b], w=[b])
        return t, b
    mu_b, mu_bb = bcast("mu", RWC)
    kkp_b, kkp_bb = bcast("k_k", 64)
    ka_b, ka_bb = bcast("k_a", 64)
    rk_b, rk_bb = bcast("r_k", 64, 0.5)
    lnw_b, lnw_bb = bcast("lnx_w", 64)
    lnb_b, lnb_bb = bcast("lnx_b", 64)
    bias_b = [bcast(f"bias{d}", 128) for d in range(2)]
    up_bd = [load_const_p(c, pd[f"up_bd{d}"], [128, 128], f"up_bd{d}") for d in range(2)]
    g_up = load_const_p(c, pd["g_up"], [128, 64], "g_up")
    osp = nc.dram_tensor("rw_osp", [NB, 2, T, 64], F32, kind="Internal").ap()
    vsp = nc.dram_tensor("rw_vsp", [NB, T, 64], F32, kind="Internal").ap()
    gsp = nc.dram_tensor("rw_gsp", [NB, T, 64], F32, kind="Internal").ap()
    bsp = nc.dram_tensor("rw_bsp", [NB, 2, T], F32, kind="Internal").ap()
    spb = [[[Buf() for _ in range(NT)] for _ in range(2)] for _ in range(NB)]
    banks = [c.ps([128, 512], F32, f"rw_bank{i}") for i in range(8)]
    bankb = [Buf(psum=True) for _ in range(8)]

    def pass_gen(b, d, pi):
        tg = f"_{b}{d}"
        bA, bB = banks[2 * pi], banks[2 * pi + 1]
        bAb, bBb = bankb[2 * pi], bankb[2 * pi + 1]
        NCOL = 448 if d == 0 else 320
        zc = c.sb([128, 448], F32, "zc" + tg)
        zp = c.sb([128, 448], F32, "zp" + tg)
        zn = c.sb([128, 448], F32, "zn" + tg)
        zm = c.sb([128, 448], F32, "zm" + tg)
        mt = c.sb([128, 448], F32, "mt" + tg)
        zinb, zmb, mtb = Buf(), Buf(), Buf()
        mub = c.sb([128, 448], F32, "mub" + tg)
        mubb = Buf()
        P.copy(mub[:, 0:192], mu_b[:, 0:192], r=[mu_bb], w=[mubb])
        if d == 0:
            P.copy(mub[:, 192:448], mu_b[:, 192:320], r=[mu_bb], w=[mubb]) if False else None
            P.copy(mub[:, 192:320], mu_b[:, 192:320], r=[mu_bb], w=[mubb])
            P.copy(mub[:, 320:448], mu_b[:, 448:576], r=[mu_bb], w=[mubb])
        else:
            P.copy(mub[:, 192:320], mu_b[:, 320:448], r=[mu_bb], w=[mubb])
        twa = c.sb([128, 128], F32, "twa" + tg)
        sgd = c.sb([128, 128], F32, "sgd" + tg)
        twab, sgdb = Buf(), Buf()
        wa = c.sb([128, 128], F32, "wa" + tg)
        sig = c.sb([128, 128], F32, "sig" + tg)
        wab, sigb = Buf(), Buf()
        gt_ = c.sb([128, 64], F32, "gt" + tg)
        gtb = Buf()
        logw = c.sb([128, 64], F32, "logw" + tg)
        logwb = Buf()
        kk0 = c.sb([128, 64], F32, "kk0" + tg)
        kk = c.sb([128, 64], F32, "kk" + tg)
        junk = c.sb([128, 64], F32, "junk" + tg)
        ss = c.sb([128, 1], F32, "ss" + tg)
        kk0b, kkb, junkb, ssb = Buf(), Buf(), Buf(), Buf()
        uu = c.sb([128, 64], F32, "uu" + tg)
        kdir = c.sb([128, 64], F32, "kdir" + tg)
        nakk = c.sb([128, 64], F32, "nakk" + tg)
        uub, kdirb, nakkb = Buf(), Buf(), Buf()
        bt1 = c.sb([128, 64], F32, "bt1" + tg)
        bs = c.sb([128, 1], F32, "bs" + tg)
        bt1b, bsb = Buf(), Buf()
        e12 = c.sb([64, 256], F32, "e12" + tg)
        e3 = c.sb([64, 128], F32, "e3" + tg)
        ecc = c.sb([64, 2], F32, "ecc" + tg)
        e4 = c.sb([128, 64], F32, "e4" + tg)
        e12b, e3b, eccb, e4b = Buf(), Buf(), Buf(), Buf()
        FM = c.sb([64, 256], F32, "FM" + tg)
        FMu = c.sb([64, 256], F32, "FMu" + tg)
        AK = c.sb([64, 256], F32, "AK" + tg)
        FMb, FMub, AKb = Buf(), Buf(), Buf()
        AH = c.sb([128, 64], F32, "AH" + tg)
        KH = c.sb([128, 64], F32, "KH" + tg)
        AHb, KHb = Buf(), Buf()
        LM = c.sb([128, 512], F32, "LM" + tg)
        L1 = c.sb([128, 128], F32, "L1" + tg)
        LMb, L1b = Buf(), Buf()
        AB = [c.sb([128, 256], F32, f"AB{i}" + tg) for i in range(2)]
        ABb = [Buf(), Buf()]
        Wt = [c.sb([128, 128], F32, f"W{i}" + tg) for i in range(2)]
        Wtb = [Buf(), Buf()]
        H = [c.sb([64, 64], F32, f"H{i}" + tg) for i in range(2)]
        Hb = [Buf(), Buf()]
        Xs = c.sb([128, 64], F32, "Xs" + tg)
        Us = c.sb([128, 64], F32, "Us" + tg)
        Os = c.sb([128, 64], F32, "Os" + tg)
        Xsb, Usb, Osb = Buf(), Buf(), Buf()
        ME1, ME1b = K[("ME1", d)]
        ME2, ME2b = K[("ME2", d)]
        MSEL, MSELb = K[("MSEL", d)]
        ME4, ME4b = K[("ME4", d)]
        MASK4, MASK4b = K[("MASK4", d)]
        STRT, STRTb = K[("STRT", d)]
        ubd, ubdb = up_bd[d]
        bia, biab = bias_b[d]
        P.memset(H[0][:, :], 0.0, w=[Hb[0]])
        hc = 0
        lo_src = 192 if d == 0 else 320
        for step in range(NT):
            ti = step if d == 0 else NT - 1 - step
            r0 = ti * 128
            for (dst, off) in ((zp, 0), (zc, 1), (zn, 2)):
                P.dma(dst[:, 0:192], zr_d[b, r0 + off:r0 + off + 128, 0:192], w=[zinb])
                P.dma(dst[:, 192:320], zr_d[b, r0 + off:r0 + off + 128, lo_src:lo_src + 128],
                      w=[zinb])
                if d == 0:
                    P.dma(dst[:, 320:448], zr_d[b, r0 + off:r0 + off + 128, 448:576], w=[zinb])
            yield
            N_ = NCOL
            P.tt(mt[:, :N_], zp[:, :N_], zn[:, :N_], ALU.add, r=[zinb], w=[mtb], eng="gpsimd")
            P.stt(mt[:, :N_], mt[:, :N_], 0.5, zc[:, :N_], ALU.mult, ALU.subtract, r=[mtb, zinb],
                  w=[mtb])
            P.tt(mt[:, :N_], mt[:, :N_], mub[:, :N_], ALU.mult, r=[mtb, mubb], w=[mtb], eng="gpsimd")
            P.tt(zm[:, :N_], zc[:, :N_], mt[:, :N_], ALU.add, r=[zinb, mtb], w=[zmb])
            rr, kc, vv = zm[:, 0:64], zm[:, 64:128], zm[:, 128:192]
            if d == 0:
                P.dma(vsp[b, r0:r0 + 128, :], vv, r=[zmb], w=[spb[b][0][ti]], eng="gpsimd")
            yield
            P.tr(bA[:, 0:128], zm[:, 192:320], ident[:, :], r=[zmb, identb], w=[bAb])
            if d == 0:
                P.tr(bA[:, 128:256], zm[:, 320:448], ident[:, :], r=[zmb, identb], w=[bAb])
            P.act(twa[0:64, :], bA[0:64, 0:128], AF.Tanh, r=[bAb], w=[twab])
            P.copy(twa[64:128, :], bA[64:128, 0:128], r=[bAb], w=[twab])
            if d == 0:
                P.act(sgd[:, :], bA[:, 128:256], AF.Sigmoid, r=[bAb], w=[sgdb])
            yield
            P.mm(bB[:, 0:128], twa[:, :], ubd[:, :], r=[twab, ubdb], w=[bBb])
            if d == 0:
                P.mm(bB[:, 128:192], sgd[:, :], g_up[0][:, :], r=[sgdb, g_up[1]], w=[bBb])
            P.tt(wa[:, :], bB[:, 0:128], bia[:, :], ALU.add, r=[bBb, biab], w=[wab])
            if d == 0:
                P.copy(gt_[:, :], bB[:, 128:192], r=[bBb], w=[gtb], eng="scalar")
                P.dma(gsp[b, r0:r0 + 128, :], gt_[:, :], r=[gtb], w=[spb[b][0][ti]], eng="gpsimd")
            P.act(sig[:, :], wa[:, :], AF.Sigmoid, r=[wab], w=[sigb])
            P.ts(logw[:, :], sig[:, 0:64], LOGW_SCALE, ALU.mult, r=[sigb], w=[logwb])
            aa = sig[:, 64:128]
            P.tt(kk0[:, :], kc, kkp_b[:, :], ALU.mult, r=[zmb, kkp_bb], w=[kk0b], eng="gpsimd")
            P.act(junk[:, :], kk0[:, :], AF.Square, r=[kk0b], w=[junkb, ssb], accum_out=ss[:, 0:1])
            P.act(ss[:, :], ss[:, :], AF.Sqrt, r=[ssb], w=[ssb])
            P.ts(ss[:, :], ss[:, :], 1e-12, ALU.max, r=[ssb], w=[ssb])
            P.op("vector", lambda e, ss=ss: e.reciprocal(out=ss[:, :], in_=ss[:, :]), r=[ssb], w=[ssb])
            P.ts(kk[:, :], kk0[:, :], ss[:, 0:1], ALU.mult, r=[kk0b, ssb], w=[kkb])
            P.stt(uu[:, :], aa, -1.0, ka_b[:, :], ALU.add, ALU.mult, r=[sigb, ka_bb], w=[uub])
            P.stt(kdir[:, :], uu[:, :], 1.0, kc, ALU.add, ALU.mult, r=[uub, zmb], w=[kdirb])
            P.stt(nakk[:, :], aa, -1.0, kk[:, :], ALU.mult, ALU.mult, r=[sigb, kkb], w=[nakkb])
            P.tt(bt1[:, :], kdir[:, :], rr, ALU.mult, r=[kdirb, zmb], w=[bt1b], eng="gpsimd")
            P.tt(bt1[:, :], bt1[:, :], rk_b[:, :], ALU.mult, r=[bt1b, rk_bb], w=[bt1b], eng="gpsimd")
            P.op("vector", lambda e, bs=bs, bt1=bt1: e.reduce_sum(out=bs[:, :], in_=bt1[:, :],
                                                                  axis=AX.X), r=[bt1b], w=[bsb])
            P.dma(bsp[b, d, r0:r0 + 128].rearrange("(p o) -> p o", o=1), bs[:, :], r=[bsb],
                  w=[spb[b][d][ti]], eng="gpsimd")
            yield
            P.mm(bA[0:64, 0:128], logw[:, :], ME1[:, :], r=[logwb, ME1b], w=[bAb])
            P.mm(bA[0:64, 128:256], logw[:, :], ME2[:, :], r=[logwb, ME2b], w=[bAb])
            P.mm(bA[0:64, 256:258], logw[:, :], MSEL[:, :], r=[logwb, MSELb], w=[bAb])
            P.mm(bA[:, 260:324], ME4[:, :], logw[:, :], r=[logwb, ME4b], w=[bAb])
            P.act(e12[:, :], bA[0:64, 0:256], AF.Exp, r=[bAb], w=[e12b])
            P.act(e3[:, :], bA[0:64, 0:128], AF.Exp, r=[bAb], w=[e3b], scale=-1.0)
            P.act(ecc[:, :], bA[0:64, 256:258], AF.Exp, r=[bAb], w=[eccb])
            P.act(e4[:, :], bA[:, 260:324], AF.Exp, r=[bAb], w=[e4b])
            yield
            P.tr(bB[0:64, 0:128], rr, ident[:, :], r=[zmb, identb], w=[bBb])
            P.tr(bB[0:64, 128:256], kk[:, :], ident[:, :], r=[kkb, identb], w=[bBb])
            P.tr(bB[0:64, 256:384], nakk[:, :], ident[:, :], r=[nakkb, identb], w=[bBb])
            P.tr(bB[0:64, 384:512], kdir[:, :], ident[:, :], r=[kdirb, identb], w=[bBb])
            P.tt(FM[:, :], bB[0:64, 0:256], e12[:, :], ALU.mult, r=[bBb, e12b], w=[FMb])
            P.tt(AK[:, 0:128], bB[0:64, 256:384], e3[:, :], ALU.mult, r=[bBb, e3b], w=[AKb])
            P.tt(AK[:, 128:256], bB[0:64, 384:512], e3[:, :], ALU.mult, r=[bBb, e3b], w=[AKb])
            P.ts(FMu[:, :], FM[:, :], ecc[:, 0:1], ALU.mult, r=[FMb, eccb], w=[FMub])
            P.tt(AH[:, :], nakk[:, :], e4[:, :], ALU.mult, r=[nakkb, e4b], w=[AHb], eng="gpsimd")
            P.tt(KH[:, :], kdir[:, :], e4[:, :], ALU.mult, r=[kdirb, e4b], w=[KHb], eng="gpsimd")
            yield
            Rt, Bt = FM[:, 0:128], FM[:, 128:256]
            At, Kt = AK[:, 0:128], AK[:, 128:256]
            P.mm(bA[:, 0:256], Kt, FM[:, :], r=[AKb, FMb], w=[bAb])
            P.mm(bA[:, 256:512], At, FM[:, :], r=[AKb, FMb], w=[bAb])
            P.mm(bB[:, 0:128], Bt, At, r=[AKb, FMb], w=[bBb])
            P.tt(LM[:, :], bA[:, :], MASK4[:, :], ALU.mult, r=[bAb, MASK4b], w=[LMb])
            P.tt(L1[:, :], bB[:, 0:128], STRT[:, :], ALU.mult, r=[bBb, STRTb], w=[L1b])
            M2T, L2T, M1T, Nm = LM[:, 0:128], LM[:, 128:256], LM[:, 256:384], LM[:, 384:512]
            P.tt(Wt[0][:, :], Nm, ident[:, :], ALU.add, r=[LMb, identb], w=[Wtb[0]], eng="gpsimd")
            yield
            for j in range(1, 8):
                bk, bkb = (bA, bAb) if j % 2 else (bB, bBb)
                if j == 1:
                    Ap, Bp, rdp = L1[:, :], Nm, [L1b, LMb]
                else:
                    Ap, Bp, rdp = AB[(j - 1) % 2][:, 0:128], AB[(j - 1) % 2][:, 128:256], \
                        [ABb[(j - 1) % 2]]
                if j <= 6:
                    P.mm(bk[:, 0:128], Bp, Ap, r=rdp, w=[bkb])
                if j <= 5:
                    P.mm(bk[:, 128:256], Ap, Bp, r=rdp, w=[bkb])
                if j >= 2:
                    wi = (j - 2) % 2
                    P.mm(bk[:, 256:384], Ap, Wt[wi][:, :], r=rdp + [Wtb[wi]], w=[bkb])
                if j <= 6:
                    ncols = 256 if j <= 5 else 128
                    P.copy(AB[j % 2][:, 0:ncols], bk[:, 0:ncols], r=[bkb], w=[ABb[j % 2]],
                           eng="scalar")
                if j >= 2:
                    wi = (j - 2) % 2
                    P.tt(Wt[1 - wi][:, :], bk[:, 256:384], Wt[wi][:, :], ALU.add,
                         r=[bkb, Wtb[wi]], w=[Wtb[1 - wi]])
                yield
            W6, W6b = Wt[0], Wtb[0]
            Hc, Hcb = H[hc], Hb[hc]
            Hn, Hnb = H[1 - hc], Hb[1 - hc]
            Rtu, Btu = FMu[:, 0:128], FMu[:, 128:256]
            P.mm(bA[:, 0:64], L2T, vv, start=True, stop=False, r=[LMb, zmb], w=[bAb])
            P.mm(bA[:, 0:64], Btu, Hc[:, :], start=False, stop=True, r=[FMub, Hcb], w=[bAb])
            P.copy(Xs[:, :], bA[:, 0:64], r=[bAb], w=[Xsb])
            P.mm(bB[:, 0:64], W6[:, :], Xs[:, :], r=[W6b, Xsb], w=[bBb])
            P.copy(Us[:, :], bB[:, 0:64], r=[bBb], w=[Usb], eng="scalar")
            P.mm(bA[0:64, 64:128], KH[:, :], vv, start=True, stop=False, r=[KHb, zmb], w=[bAb])
            P.mm(bA[0:64, 64:128], AH[:, :], Us[:, :], start=False, stop=True, r=[AHb, Usb], w=[bAb])
            P.mm(bA[:, 128:192], M2T, vv, start=True, stop=False, r=[LMb, zmb], w=[bAb])
            P.mm(bA[:, 128:192], Rtu, Hc[:, :], start=False, stop=False, r=[FMub, Hcb], w=[bAb])
            P.mm(bA[:, 128:192], M1T, Us[:, :], start=False, stop=True, r=[LMb, Usb], w=[bAb])
            P.stt(Hn[:, :], Hc[:, :], ecc[:, 1:2], bA[0:64, 64:128], ALU.mult, ALU.add,
                  r=[Hcb, eccb, bAb], w=[Hnb])
            P.copy(Os[:, :], bA[:, 128:192], r=[bAb], w=[Osb], eng="scalar")
            P.dma(osp[b, d, r0:r0 + 128, :], Os[:, :], r=[Osb], w=[spb[b][d][ti]], eng="gpsimd")
            hc = 1 - hc
            yield

    gens = []
    pi = 0
    for b in range(NB):
        for d in range(2):
            gens.append(pass_gen(b, d, pi % 4))
            pi += 1
    grp = 4
    for g0 in range(0, len(gens), grp):
        live = gens[g0:g0 + grp]
        for k, g in enumerate(live):
            for _ in range(4 * k):
                next(g)
        while live:
            for g in list(live):
                try:
                    next(g)
                except StopIteration:
                    live.remove(g)
    TBK = min(NT, 16)
    of = c.sb([128, TBK, 64], F32, "ep_of")
    ob = c.sb([128, TBK, 64], F32, "ep_ob")
    vt = c.sb([128, TBK, 64], F32, "ep_v")
    gt2 = c.sb([128, TBK, 64], F32, "ep_g")
    b0 = c.sb([128, TBK], F32, "ep_b0")
    b1 = c.sb([128, TBK], F32, "ep_b1")
    st = c.sb([128, TBK], F32, "ep_st")
    sq2 = c.sb([128, TBK, 64], F32, "ep_sq")
    epb = Buf()
    for b in range(NB):
        for k0 in range(0, NT, TBK):
            rs_ = slice(k0 * 128, (k0 + TBK) * 128)
            deps = [spb[b][d][ti] for d in range(2) for ti in range(k0, k0 + TBK)]
            pat = "(j p) v -> p j v"
            P.dma(of[:, :, :], osp[b, 0, rs_, :].rearrange(pat, p=128), r=deps, w=[epb])
            P.dma(ob[:, :, :], osp[b, 1, rs_, :].rearrange(pat, p=128), r=deps, w=[epb])
            P.dma(vt[:, :, :], vsp[b, rs_, :].rearrange(pat, p=128), r=deps, w=[epb])
            P.dma(gt2[:, :, :], gsp[b, rs_, :].rearrange(pat, p=128), r=deps, w=[epb])
            P.dma(b0[:, :], bsp[b, 0, rs_].rearrange("(j p) -> p j", p=128), r=deps, w=[epb],
                  allow_slow_non_contiguous=True)
            P.dma(b1[:, :], bsp[b, 1, rs_].rearrange("(j p) -> p j", p=128), r=deps, w=[epb],
                  allow_slow_non_contiguous=True)
            E = [epb]
            bc = lambda t_: t_[:, :].unsqueeze(2).to_broadcast([128, TBK, 64])
            P.tt(of[:, :, :], of[:, :, :], ob[:, :, :], ALU.add, r=E, w=E)
            P.op("vector", lambda e: e.reduce_sum(out=st[:, :], in_=of[:, :, :], axis=AX.X), r=E, w=E)
            P.ts(st[:, :], st[:, :], 1.0 / 64, ALU.mult, r=E, w=E)
            P.tt(of[:, :, :], of[:, :, :], bc(st), ALU.subtract, r=E, w=E)
            P.tt(sq2[:, :, :], of[:, :, :], of[:, :, :], ALU.mult, r=E, w=E)
            P.op("vector", lambda e: e.reduce_sum(out=st[:, :], in_=sq2[:, :, :], axis=AX.X), r=E, w=E)
            P.ts(st[:, :], st[:, :], 1.0 / 64, ALU.mult, GN_EPS, ALU.add, r=E, w=E)
            P.act(st[:, :], st[:, :], AF.Sqrt, r=E, w=E)
            P.op("vector", lambda e: e.reciprocal(out=st[:, :], in_=st[:, :]), r=E, w=E)
            P.tt(of[:, :, :], of[:, :, :], bc(st), ALU.mult, r=E, w=E)
            P.tt(of[:, :, :], of[:, :, :], lnw_b[:, :].unsqueeze(1).to_broadcast([128, TBK, 64]),
                 ALU.mult, r=E + [lnw_bb], w=E)
            P.tt(of[:, :, :], of[:, :, :], lnb_b[:, :].unsqueeze(1).to_broadcast([128, TBK, 64]),
                 ALU.add, r=E + [lnb_bb], w=E)
            P.tt(b0[:, :], b0[:, :], b1[:, :], ALU.add, r=E, w=E)
            P.tt(vt[:, :, :], vt[:, :, :], bc(b0), ALU.mult, r=E, w=E)
            P.tt(of[:, :, :], of[:, :, :], vt[:, :, :], ALU.add, r=E, w=E)
            P.tt(of[:, :, :], of[:, :, :], gt2[:, :, :], ALU.mult, r=E, w=E)
            P.dma(y_d[b, rs_, :].rearrange(pat, p=128), of[:, :, :], r=E, is_out=True)


def load_const_p(c, ap, shape, name):
    t = c.sb(shape, F32, "p_" + name)
    b = Buf()
    c.P.dma(t[:, :], ap[:, :], w=[b])
    return t, b


RW_PARAM_SHAPES = {"mu": [RWC], "up_bd0": [128, 128], "up_bd1": [128, 128], "bias0": [128],
                   "bias1": [128], "g_up": [128, 64], "k_k": [64], "k_a": [64], "r_k": [64],
                   "lnx_w": [64], "lnx_b": [64]}


def build_rwkv(T, NB):
    c = Ctx()
    zr_d = c.din("zr", [NB, T + 2, RWC])
    pd = {k: c.din("rwp_" + k, shp) for k, shp in RW_PARAM_SHAPES.items()}
    cd = {"ident": c.din("rwc_ident", [128, 128])}
    for d in range(2):
        for k, shp in RW_CONST_SHAPES.items():
            cd[f"{k}{d}"] = c.din(f"rwc_{k}{d}", shp)
    y_d = c.dout("y_rw", [NB, T, 64])
    emit_rwkv(c, T, NB, zr_d, pd, cd, y_d)
    return c.finish()


def rwkv_in_map(h, z_rw, params):
    p = rwkv_host_params(h, *params)
    cols = p.pop("rw_cols")
    NB, T, _ = z_rw.shape
    zr = np.zeros((NB, T + 2, RWC), np.float32)
    zr[:, 1:T + 1, :] = z_rw[:, :, cols]
    m = {"zr": zr}
    for k, v in p.items():
        m["rwp_" + k] = np.ascontiguousarray(v, dtype=np.float32)
    for k, v in rwkv_consts().items():
        m["rwc_" + k] = v
    return m


NEXP = 16
CAP = 1024


def load_w_bf16(c, w_ap, kc, ncols, name, eng="gpsimd"):
    P = c.P
    W = c.sb([128, kc, ncols], BF16, name)
    Wb = Buf()
    stg = [c.sb([128, ncols], F32, f"{name}_stg{i}") for i in range(2)]
    stgb = [Buf(), Buf()]
    for ch in range(kc):
        s = ch % 2
        P.dma(stg[s][:, :], w_ap[ch * 128:(ch + 1) * 128, :], w=[stgb[s]])
        P.copy(W[:, ch, :], stg[s][:, :], r=[stgb[s]], w=[Wb], eng=eng)
    return W, Wb


def build_phase_c(nt_core, tb=512):
    c = Ctx()
    P = c.P
    xT_d = c.din("xT", [D, nt_core])
    yrw_d = c.din("yrwT", [512, nt_core])
    om_d = c.din("omT", [512, nt_core])
    wout_d = c.din("w_out", [D, D])
    on_d = c.din("o_norm", [512])
    fn_d = c.din("ffn_norm", [D])
    rt_d = c.din("router", [D, NEXP])
    id_d = c.din("ident", [128, 128])
    x1_d = c.dout("x1T", [D, nt_core])
    h2_d = c.dout("h2", [nt_core, D], BF16)
    aff_d = c.dout("aff", [nt_core, NEXP])

    ident, identb = load_const_p(c, id_d, [128, 128], "identc")
    og, ogb = load_col_vec(c, on_d, 512, "og")
    fg, fgb = load_col_vec(c, fn_d, D, "fg")
    ones = c.sb([128, 128], F32, "ones")
    onesb = Buf()
    P.memset(ones[:, :], 1.0, w=[onesb])
    Wo, Wob = load_w_bf16(c, wout_d, KC, D, "Wo")
    rt = c.sb([128, KC, NEXP], F32, "rt")
    rtb = Buf()
    P.dma(rt[:, :, :], rt_d.rearrange("(c p) e -> p c e", p=128), w=[rtb])
    banks = [c.ps([128, 512], F32, f"c_bank{i}") for i in range(8)]
    bankb = [Buf(psum=True) for _ in range(8)]
    xT = c.sb([128, KC, tb], F32, "xT")
    xb = Buf()
    om = c.sb([128, 4, tb], F32, "om")
    omb = Buf()
    yrw = c.sb([128, 4, tb], F32, "yrw")
    yrwb = Buf()
    ycat = c.sb([128, KC, tb], BF16, "ycat")
    ycatb = Buf()
    sq = c.sb([128, KC, tb], F32, "sq")
    sqb = Buf()
    tmp = c.sb([128, tb], F32, "tmp")
    tmpb = Buf()
    x1 = c.sb([128, KC, tb], F32, "x1")
    x1b = Buf()
    h2T = c.sb([128, KC, tb], F32, "h2T")
    h2Tb = Buf()
    lg = c.sb([128, 4, NEXP], F32, "lg")
    lgb = Buf()
    mx = c.sb([128, 4], F32, "mx")
    mxb = Buf()
    h2tok = [c.sb([128, D], BF16, f"h2tok{i}") for i in range(2)]
    h2tokb = [Buf(), Buf()]
    nblk = nt_core // tb
    for blk in range(nblk):
        t0 = blk * tb
        tsl = slice(t0, t0 + tb)
        P.dma(xT[:, :, :], xT_d[:, tsl].rearrange("(c p) t -> p c t", p=128), w=[xb])
        P.dma(om[:, :, :], om_d[:, tsl].rearrange("(c p) t -> p c t", p=128), w=[omb])
        P.dma(yrw[:, :, :], yrw_d[:, tsl].rearrange("(c p) t -> p c t", p=128), w=[yrwb])
        P.copy(ycat[:, 0:4, :], yrw[:, :, :], r=[yrwb], w=[ycatb], eng="gpsimd")
        fm_rmsnorm(c, om, omb, og, ogb, ycat[:, 4:8, :], ycatb, ones, onesb, banks[0], bankb[0],
                   tmp, tmpb, tb, kc=4, dim=512, sq=sq, sqb=sqb)
        for oc in range(KC):
            q = 1 + oc % 3
            for ch in range(KC):
                P.mm(banks[q][:, :], Wo[:, ch, oc * 128:(oc + 1) * 128], ycat[:, ch, :],
                     start=(ch == 0), stop=(ch == KC - 1), r=[Wob, ycatb], w=[bankb[q]])
            P.tt(x1[:, oc, :], banks[q][:, :], xT[:, oc, :], ALU.add, r=[bankb[q], xb], w=[x1b])
        P.dma(x1_d[:, tsl].rearrange("(c p) t -> p c t", p=128), x1[:, :, :], r=[x1b], is_out=True,
              eng="gpsimd")
        fm_rmsnorm(c, x1, x1b, fg, fgb, h2T, h2Tb, ones, onesb, banks[0], bankb[0], tmp, tmpb, tb,
                   sq=sq, sqb=sqb)
        for sub in range(4):
            for ch in range(KC):
                P.mm(banks[4][:, sub * NEXP:(sub + 1) * NEXP], h2T[:, ch, sub * 128:(sub + 1) * 128],
                     rt[:, ch, :], start=(ch == 0), stop=(ch == KC - 1), r=[h2Tb, rtb],
                     w=[bankb[4]])
        lgv = banks[4][:, 0:4 * NEXP].rearrange("p (s e) -> p s e", e=NEXP)
        P.op("vector", lambda e, lgv=lgv: e.reduce_max(out=mx[:, :], in_=lgv, axis=AX.X),
             r=[bankb[4]], w=[mxb])
        P.tt(lg[:, :, :], lgv, mx[:, :].unsqueeze(2).to_broadcast([128, 4, NEXP]), ALU.subtract,
             r=[bankb[4], mxb], w=[lgb])
        P.act(lg[:, :, :], lg[:, :, :], AF.Exp, r=[lgb], w=[lgb])
        P.op("vector", lambda e: e.reduce_sum(out=mx[:, :], in_=lg[:, :, :], axis=AX.X), r=[lgb],
             w=[mxb])
        P.op("vector", lambda e: e.reciprocal(out=mx[:, :], in_=mx[:, :]), r=[mxb], w=[mxb])
        P.tt(lg[:, :, :], lg[:, :, :], mx[:, :].unsqueeze(2).to_broadcast([128, 4, NEXP]), ALU.mult,
             r=[lgb, mxb], w=[lgb])
        P.dma(aff_d[tsl, :].rearrange("(s p) e -> p s e", p=128), lg[:, :, :], r=[lgb], is_out=True,
              eng="gpsimd")
        for sub in range(4):
            s2 = sub % 2
            for ch in range(KC):
                q = 5 + ch // 4
                P.tr(banks[q][:, (ch % 4) * 128:(ch % 4 + 1) * 128], h2T[:, ch, sub * 128:(sub + 1) * 128],
                     ident[:, :], r=[h2Tb, identb], w=[bankb[q]])
            P.copy(h2tok[s2][:, 0:512], banks[5][:, :], r=[bankb[5]], w=[h2tokb[s2]], eng="scalar")
            P.copy(h2tok[s2][:, 512:1024], banks[6][:, :], r=[bankb[6]], w=[h2tokb[s2]])
            P.dma(h2_d[t0 + sub * 128:t0 + (sub + 1) * 128, :], h2tok[s2][:, :], r=[h2tokb[s2]],
                  is_out=True, eng="gpsimd")
    return c.finish()


def phase_d_consts(T):
    idx = np.arange(128)
    tris = (idx[:, None] < idx[None, :]).astype(np.float32)
    nj = T // 128
    j = np.arange(nj)
    trij = (j[:, None] < j[None, :]).astype(np.float32)
    iota = np.tile(np.arange(CAP, dtype=np.float32)[None, :], (128, 1))
    return {"tris": tris, "trij": trij, "iota": iota, "ones": np.ones((128, 128), np.float32),
            "ident": np.eye(128, dtype=np.float32)}


def build_phase_d(T, NB, n_iter=34):
    c = Ctx()
    P = c.P
    NJ = T // 128
    NPR = 2 * NB
    aff_d = c.din("affT", [2, NB, T])
    h2_d = c.din("h2", [NB, T, D], BF16)
    wg_d = c.din("w_gate", [2, D, D])
    wu_d = c.din("w_up", [2, D, D])
    wd_d = c.din("w_down", [2, D, D])
    tris_d = c.din("tris", [128, 128])
    trij_d = c.din("trij", [NJ, NJ])
    iota_d = c.din("iota", [128, CAP])
    ones_d = c.din("ones", [128, 128])
    ye_d = c.dout("ye", [2, NB, CAP, D], BF16)
    posm_d = c.dout("posm", [2, NB, T])

    tris, trisb = load_const_p(c, tris_d, [128, 128], "tris")
    trij, trijb = load_const_p(c, trij_d, [NJ, NJ], "trij")
    iota, iotab = load_const_p(c, iota_d, [128, CAP], "iota")
    ones, onesb = load_const_p(c, ones_d, [128, 128], "onesd")
    banks = [c.ps([128, 512], F32, f"d_bank{i}") for i in range(8)]
    bankb = [Buf(psum=True) for _ in range(8)]
    afft = c.sb([128, NPR, NJ], F32, "afft")
    afftb = Buf()
    for e in range(2):
        for b in range(NB):
            P.dma(afft[:, e * NB + b, :], aff_d[e, b, :].rearrange("(j p) -> p j", p=128), w=[afftb],
                  allow_slow_non_contiguous=True)
    lo = c.sb([128, NPR], F32, "lo")
    hi = c.sb([128, NPR], F32, "hi")
    mid = c.sb([128, NPR], F32, "mid")
    cnt = c.sb([128, NPR], F32, "cnt")
    ge = c.sb([128, NPR], F32, "ge")
    dl = c.sb([128, NPR], F32, "dl")
    cmpj = c.sb([128, NPR, NJ], F32, "cmpj")
    B_ = {k: Buf() for k in ("lo", "hi", "mid", "cnt", "ge", "dl", "cmp")}
    P.memset(lo[:, :], 0.0, w=[B_["lo"]])
    P.memset(hi[:, :], 1.0001, w=[B_["hi"]])
    P.memset(mid[:, :], 0.5, w=[B_["mid"]])
    for it in range(n_iter):
        P.tt(cmpj[:, :, :], afft[:, :, :], mid[:, :].unsqueeze(2).to_broadcast([128, NPR, NJ]),
             ALU.is_ge, r=[afftb, B_["mid"]], w=[B_["cmp"]])
        P.op("vector", lambda e: e.reduce_sum(out=cnt[:, :], in_=cmpj[:, :, :], axis=AX.X),
             r=[B_["cmp"]], w=[B_["cnt"]])
        P.mm(banks[0][:, 0:NPR], ones[:, :], cnt[:, :], r=[onesb, B_["cnt"]], w=[bankb[0]])
        P.ts(ge[:, :], banks[0][:, 0:NPR], float(CAP), ALU.is_ge, r=[bankb[0]], w=[B_["ge"]])
        P.tt(dl[:, :], mid[:, :], lo[:, :], ALU.subtract, r=[B_["mid"], B_["lo"]], w=[B_["dl"]])
        P.tt(dl[:, :], dl[:, :], ge[:, :], ALU.mult, r=[B_["dl"], B_["ge"]], w=[B_["dl"]])
        P.tt(lo[:, :], lo[:, :], dl[:, :], ALU.add, r=[B_["lo"], B_["dl"]], w=[B_["lo"]])
        P.tt(dl[:, :], hi[:, :], mid[:, :], ALU.subtract, r=[B_["hi"], B_["mid"]], w=[B_["dl"]])
        P.tt(dl[:, :], dl[:, :], ge[:, :], ALU.mult, r=[B_["dl"], B_["ge"]], w=[B_["dl"]])
        P.tt(hi[:, :], mid[:, :], dl[:, :], ALU.add, r=[B_["mid"], B_["dl"]], w=[B_["hi"]])
        P.tt(mid[:, :], lo[:, :], hi[:, :], ALU.add, r=[B_["lo"], B_["hi"]], w=[B_["mid"]])
        P.ts(mid[:, :], mid[:, :], 0.5, ALU.mult, r=[B_["mid"]], w=[B_["mid"]])
    mask = c.sb([128, NPR, NJ], F32, "mask")
    maskb = Buf()
    P.tt(mask[:, :, :], afft[:, :, :], lo[:, :].unsqueeze(2).to_broadcast([128, NPR, NJ]),
         ALU.is_ge, r=[afftb, B_["lo"]], w=[maskb])
    totT = c.sb([NJ, NPR, 128], F32, "totT")
    totTb = Buf()
    pos = c.sb([128, NPR, NJ], F32, "pos")
    posb = Buf()
    posm = c.sb([128, NPR, NJ], F32, "posm")
    posmb = Buf()
    for pr in range(NPR):
        P.mm(banks[1][0:NJ, 0:128], mask[:, pr, :], ones[:, :], r=[maskb, onesb], w=[bankb[1]])
        P.copy(totT[:, pr, :], banks[1][0:NJ, 0:128], r=[bankb[1]], w=[totTb])
        P.mm(banks[2][:, 0:NJ], tris[:, :], mask[:, pr, :], start=True, stop=False,
             r=[trisb, maskb], w=[bankb[2]])
        P.mm(banks[2][:, 0:NJ], totT[:, pr, :], trij[:, :], start=False, stop=True,
             r=[totTb, trijb], w=[bankb[2]])
        P.copy(pos[:, pr, :], banks[2][:, 0:NJ], r=[bankb[2]], w=[posb])
    P.stt(posm[:, :, :], pos[:, :, :], 1.0, mask[:, :, :], ALU.add, ALU.mult, r=[posb, maskb],
          w=[posmb])
    P.ts(posm[:, :, :], posm[:, :, :], -1.0, ALU.add, r=[posmb], w=[posmb])
    for e in range(2):
        for b in range(NB):
            P.dma(posm_d[e, b, :].rearrange("(j p) -> p j", p=128), posm[:, e * NB + b, :],
                  r=[posmb], is_out=True, eng="gpsimd", allow_slow_non_contiguous=True)
    OH = c.sb([128, NJ, 512], BF16, "OH")
    OHb = Buf()
    h2t = [c.sb([128, D], BF16, f"h2t{i}") for i in range(6)]
    h2tb = [Buf() for _ in range(6)]
    xeT = c.sb([128, KC, CAP], BF16, "xeT")
    xeTb = Buf()
    hidT = c.sb([128, KC, CAP], BF16, "hidT")
    hidTb = Buf()
    sg = [c.sb([128, 512], F32, f"sg{i}") for i in range(2)]
    sgb = [Buf(), Buf()]
    yo = [c.sb([128, D], BF16, f"yo{i}") for i in range(2)]
    yob = [Buf(), Buf()]
    Wg = c.sb([128, KC, D], BF16, "Wg")
    Wu = c.sb([128, KC, D], BF16, "Wu")
    Wd = c.sb([128, KC, D], BF16, "Wd")
    Wgb, Wub, Wdb = Buf(), Buf(), Buf()
    stg = [c.sb([128, D], F32, f"wstg{i}") for i in range(2)]
    stgb = [Buf(), Buf()]
    sk = 0
    for e in range(2):
        for (W, Wb, wd_) in ((Wg, Wgb, wg_d), (Wu, Wub, wu_d), (Wd, Wdb, wd_d)):
            for ch in range(KC):
                s = sk % 2
                sk += 1
                P.dma(stg[s][:, :], wd_[e, ch * 128:(ch + 1) * 128, :], w=[stgb[s]])
                P.copy(W[:, ch, :], stg[s][:, :], r=[stgb[s]], w=[Wb], eng="gpsimd")
        for b in range(NB):
            pr = e * NB + b
            for half in range(2):
                for j in range(NJ):
                    P.ts(OH[:, j, :], iota[:, half * 512:(half + 1) * 512], posm[:, pr, j:j + 1],
                         ALU.is_equal, r=[iotab, posmb], w=[OHb])
                for j in range(NJ):
                    s3 = j % 6
                    P.dma(h2t[s3][:, :], h2_d[b, j * 128:(j + 1) * 128, :], w=[h2tb[s3]])
                    for ch in range(KC):
                        P.mm(banks[ch][:, :], h2t[s3][:, ch * 128:(ch + 1) * 128], OH[:, j, :],
                             start=(j == 0), stop=(j == NJ - 1), r=[h2tb[s3], OHb], w=[bankb[ch]])
                for ch in range(KC):
                    P.copy(xeT[:, ch, half * 512:(half + 1) * 512], banks[ch][:, :], r=[bankb[ch]],
                           w=[xeTb], eng=("scalar" if ch % 2 else "vector"))
            k = 0
            for fc in range(KC):
                for half in range(2):
                    hs = slice(half * 512, (half + 1) * 512)
                    qg, qu = 2 * (k % 4), 2 * (k % 4) + 1
                    s2 = k % 2
                    k += 1
                    for ch in range(KC):
                        P.mm(banks[qg][:, :], Wg[:, ch, fc * 128:(fc + 1) * 128], xeT[:, ch, hs],
                             start=(ch == 0), stop=(ch == KC - 1), r=[Wgb, xeTb], w=[bankb[qg]])
                    for ch in range(KC):
                        P.mm(banks[qu][:, :], Wu[:, ch, fc * 128:(fc + 1) * 128], xeT[:, ch, hs],
                             start=(ch == 0), stop=(ch == KC - 1), r=[Wub, xeTb], w=[bankb[qu]])
                    P.act(sg[s2][:, :], banks[qg][:, :], AF.Silu, r=[bankb[qg]], w=[sgb[s2]])
                    P.tt(hidT[:, fc, hs], banks[qu][:, :], sg[s2][:, :], ALU.mult,
                         r=[bankb[qu], sgb[s2]], w=[hidTb])
            for sbk in range(CAP // 128):
                s2 = sbk % 2
                for half in range(2):
                    q = (2 * sbk + half) % 8
                    for fc in range(KC):
                        P.mm(banks[q][:, :], hidT[:, fc, sbk * 128:(sbk + 1) * 128],
                             Wd[:, fc, half * 512:(half + 1) * 512], start=(fc == 0),
                             stop=(fc == KC - 1), r=[hidTb, Wdb], w=[bankb[q]])
                    P.copy(yo[s2][:, half * 512:(half + 1) * 512], banks[q][:, :], r=[bankb[q]],
                           w=[yob[s2]], eng=("scalar" if half else "vector"))
                P.dma(ye_d[e, b, sbk * 128:(sbk + 1) * 128, :], yo[s2][:, :], r=[yob[s2]],
                      is_out=True, eng="gpsimd")
    return c.finish()


def build_phase_e(nt_core, final, tb=512):
    c = Ctx()
    P = c.P
    x1_d = c.din("x1T", [D, nt_core])
    ye_d = c.din("ye", [NEXP, CAP, D], BF16)
    posm_d = c.din("posm", [NEXP, nt_core])
    aff_d = c.din("affT", [NEXP, nt_core])
    pT_d = c.din("pT", [256, nt_core])
    pn_d = c.din("ple_norm", [D])
    pp_d = c.din("ple_proj", [256, D])
    pg_d = c.din("ple_gate", [D, D])
    sid_d = c.din("slotid", [128, 8])
    fn_d = c.din("final_norm", [D])
    out_d = c.dout("x3T", [D, nt_core])

    sid, sidb = load_const_p(c, sid_d, [128, 8], "sid")
    pg, pgb = load_col_vec(c, pn_d, D, "pn")
    fg, fgb = load_col_vec(c, fn_d, D, "fn")
    ones = c.sb([128, 128], F32, "ones")
    onesb = Buf()
    P.memset(ones[:, :], 1.0, w=[onesb])
    Wgt, Wgtb = load_w_bf16(c, pg_d, KC, D, "Wpg")
    Wp, Wpb = load_w_bf16(c, pp_d, 2, D, "Wpp")
    banks = [c.ps([128, 512], F32, f"e_bank{i}") for i in range(8)]
    bankb = [Buf(psum=True) for _ in range(8)]
    x1 = c.sb([128, KC, tb], F32, "x1")
    x1b = Buf()
    x2 = c.sb([128, KC, tb], F32, "x2")
    x2b = Buf()
    h3 = c.sb([128, KC, tb], BF16, "h3")
    h3b = Buf()
    sq = c.sb([128, KC, tb], F32, "sq")
    sqb = Buf()
    tmp = c.sb([128, tb], F32, "tmp")
    tmpb = Buf()
    pT = c.sb([128, 2, tb], F32, "pT")
    pTb = Buf()
    pTh = c.sb([128, 2, tb], BF16, "pTh")
    pThb = Buf()
    posr = [c.sb([128, tb], F32, f"posr{i}") for i in range(2)]
    affr = [c.sb([128, tb], F32, f"affr{i}") for i in range(2)]
    rowb = [Buf(), Buf()]
    OHT = [c.sb([128, 8, tb], BF16, f"OHT{i}") for i in range(2)]
    OHTb = [Buf(), Buf()]
    yet = [c.sb([128, 8, D], BF16, f"yet{i}") for i in range(3)]
    yetb = [Buf(), Buf(), Buf()]
    sgm = c.sb([128, tb], F32, "sgm")
    sgmb = Buf()
    nblk = nt_core // tb
    for blk in range(nblk):
        t0 = blk * tb
        tsl = slice(t0, t0 + tb)
        P.dma(x1[:, :, :], x1_d[:, tsl].rearrange("(c p) t -> p c t", p=128), w=[x1b])
        P.dma(pT[:, :, :], pT_d[:, tsl].rearrange("(c p) t -> p c t", p=128), w=[pTb])
        for e in range(NEXP):
            s = e % 2
            P.dma(posr[s][:, :], posm_d[e, tsl].partition_broadcast(128), w=[rowb[s]])
            P.dma(affr[s][:, :], aff_d[e, tsl].partition_broadcast(128), w=[rowb[s]])
            sy = e % 3
            P.dma(yet[sy][:, :, :], ye_d[e, :, :].rearrange("(sb p) d -> p sb d", p=128),
                  w=[yetb[sy]])
            for sbk in range(8):
                P.stt(OHT[s][:, sbk, :], posr[s][:, :], sid[:, sbk:sbk + 1], affr[s][:, :],
                      ALU.is_equal, ALU.mult, r=[rowb[s], sidb], w=[OHTb[s]])
            for dc in range(KC):
                for sbk in range(8):
                    P.mm(banks[dc][:, :], yet[sy][:, sbk, dc * 128:(dc + 1) * 128], OHT[s][:, sbk, :],
                         start=(e == 0 and sbk == 0), stop=(e == NEXP - 1 and sbk == 7),
                         r=[yetb[sy], OHTb[s]], w=[bankb[dc]])
        for dc in range(KC):
            P.tt(x2[:, dc, :], banks[dc][:, :], x1[:, dc, :], ALU.add, r=[bankb[dc], x1b], w=[x2b])
        fm_rmsnorm(c, x2, x2b, pg, pgb, h3, h3b, ones, onesb, banks[0], bankb[0], tmp, tmpb, tb,
                   sq=sq, sqb=sqb)
        P.copy(pTh[:, :, :], pT[:, :, :], r=[pTb], w=[pThb], eng="gpsimd")
        for oc in range(KC):
            qa, qb = 1 + 2 * (oc % 3), 2 + 2 * (oc % 3)
            for ch in range(KC):
                P.mm(banks[qa][:, :], Wgt[:, ch, oc * 128:(oc + 1) * 128], h3[:, ch, :],
                     start=(ch == 0), stop=(ch == KC - 1), r=[Wgtb, h3b], w=[bankb[qa]])
            for ch in range(2):
                P.mm(banks[qb][:, :], Wp[:, ch, oc * 128:(oc + 1) * 128], pTh[:, ch, :],
                     start=(ch == 0), stop=(ch == 1), r=[Wpb, pThb], w=[bankb[qb]])
            P.act(sgm[:, :], banks[qa][:, :], AF.Sigmoid, r=[bankb[qa]], w=[sgmb])
            P.tt(sgm[:, :], banks[qb][:, :], sgm[:, :], ALU.mult, r=[bankb[qb], sgmb], w=[sgmb])
            P.tt(x2[:, oc, :], x2[:, oc, :], sgm[:, :], ALU.add, r=[x2b, sgmb], w=[x2b],
                 eng="gpsimd")
        if final:
            fm_rmsnorm(c, x2, x2b, fg, fgb, x1, x1b, ones, onesb, banks[0], bankb[0], tmp, tmpb, tb,
                       sq=sq, sqb=sqb)
            P.dma(out_d[:, tsl].rearrange("(c p) t -> p c t", p=128), x1[:, :, :], r=[x1b],
                  is_out=True, eng="gpsimd")
        else:
            P.dma(out_d[:, tsl].rearrange("(c p) t -> p c t", p=128), x2[:, :, :], r=[x2b],
                  is_out=True, eng="gpsimd")
    return c.finish()


_NC_CACHE = {}
_DBG = None


def _get(key, fn):
    if key not in _NC_CACHE:
        _NC_CACHE[key] = fn()
    return _NC_CACHE[key]


def _c(a):
    return np.ascontiguousarray(a)


def kernel(**inputs):
    inp = {k: np.asarray(v) for k, v in inputs.items()}
    x = inp["x"]
    B_, T_, D_ = x.shape
    L_ = inp["w_in"].shape[0]
    NTOK = B_ * T_
    NTC = NTOK // NCORES
    CPB = NCORES // B_
    xT_c = [_c(x.reshape(NTOK, D_)[i * NTC:(i + 1) * NTC].T) for i in range(NCORES)]
    mc = mla_consts()
    dc = phase_d_consts(T_)
    slotid = (np.arange(8, dtype=np.float32)[None, :] * 128 + np.arange(128, dtype=np.float32)[:, None])
    slotid = _c(slotid.astype(np.float32))
    pos = inp["positions"].astype(np.int32)
    for l in range(L_):
        ncA = _get(("A", NTC), lambda: build_phase_a(NTC))
        res = run_spmd(ncA, [{"xT": xT_c[i], "w_in": _c(inp["w_in"][l]), "g": _c(inp["attn_norm"][l])}
                             for i in range(NCORES)])
        zT = np.concatenate([r["zT"] for r in res], axis=1)
        z = zT.T.reshape(B_, T_, IN_COLS)
        if _DBG is not None:
            _DBG[f'z{l}'] = z.copy()
        zm = _c(z[:, :, 1920:].transpose(0, 2, 1))
        ncM = _get(("M", T_, B_), lambda: build_mla(T_, B_))
        q_up = inp["mla_q_up"][l]
        kv_up = inp["mla_kv_up"][l]
        maps = []
        for h in range(8):
            m = {"zm": zm, "pos": pos, "q_norm": _c(inp["mla_q_norm"][l]),
                 "kv_norm": _c(inp["mla_kv_norm"][l]), "q_up": _c(q_up[:, h * 96:(h + 1) * 96]),
                 "kv_up_k": _c(kv_up[:, h * 128:h * 128 + 64]),
                 "kv_up_v": _c(kv_up[:, h * 128 + 64:(h + 1) * 128])}
            m.update(mc)
            maps.append(m)
        resM = run_spmd(ncM, maps)
        o_mla = np.concatenate([r["o"] for r in resM], axis=2)
        ncR = _get(("R", T_, B_), lambda: build_rwkv(T_, B_))
        params = [inp[k][l] for k in ["rw_mu", "rw_w0", "rw_w_up", "rw_a0", "rw_a_up", "rw_g_up",
                                      "rw_k_k", "rw_k_a", "rw_r_k", "rw_lnx_w", "rw_lnx_b"]]
        z_rw = z[:, :, :1920]
        resR = run_spmd(ncR, [rwkv_in_map(h, z_rw, params) for h in range(8)])
        y_rw = np.concatenate([r["y_rw"] for r in resR], axis=2)
        del z, zT, zm, z_rw
        if _DBG is not None:
            _DBG[f'yrw{l}'] = y_rw
            _DBG[f'omla{l}'] = o_mla
        ncC = _get(("C", NTC), lambda: build_phase_c(NTC))
        yrwT = y_rw.reshape(NTOK, 512)
        omT = o_mla.reshape(NTOK, 512)
        maps = []
        for i in range(NCORES):
            sl = slice(i * NTC, (i + 1) * NTC)
            maps.append({"xT": xT_c[i], "yrwT": _c(yrwT[sl].T), "omT": _c(omT[sl].T),
                         "w_out": _c(inp["w_out"][l]), "o_norm": _c(inp["mla_o_norm"][l]),
                         "ffn_norm": _c(inp["ffn_norm"][l]), "router": _c(inp["router"][l]),
                         "ident": dc["ident"]})
        resC = run_spmd(ncC, maps)
        x1T_c = [r["x1T"] for r in resC]
        h2 = np.concatenate([r["h2"] for r in resC], axis=0).reshape(B_, T_, D_)
        aff = np.concatenate([r["aff"] for r in resC], axis=0)
        affT = _c(aff.reshape(B_, T_, NEXP).transpose(2, 0, 1))
        if _DBG is not None:
            _DBG[f'aff{l}'] = aff
            _DBG[f'x1T{l}'] = x1T_c
            _DBG[f'h2{l}'] = h2
        ncD = _get(("D", T_, B_), lambda: build_phase_d(T_, B_))
        maps = []
        for i in range(NCORES):
            es = slice(2 * i, 2 * i + 2)
            maps.append({"affT": _c(affT[es]), "h2": h2, "w_gate": _c(inp["exp_w_gate"][l][es]),
                         "w_up": _c(inp["exp_w_up"][l][es]), "w_down": _c(inp["exp_w_down"][l][es]),
                         "tris": dc["tris"], "trij": dc["trij"], "iota": dc["iota"],
                         "ones": dc["ones"]})
        resD = run_spmd(ncD, maps)
        ye = np.concatenate([r["ye"] for r in resD], axis=0)
        posm = np.concatenate([r["posm"] for r in resD], axis=0)
        if _DBG is not None:
            _DBG[f'posm{l}'] = posm
            _DBG[f'ye{l}'] = ye
        final = (l == L_ - 1)
        ncE = _get(("E", NTC, final), lambda: build_phase_e(NTC, final))
        maps = []
        for i in range(NCORES):
            b = i // CPB
            tsl = slice((i % CPB) * NTC, (i % CPB + 1) * NTC)
            maps.append({"x1T": x1T_c[i], "ye": _c(ye[:, b]), "posm": _c(posm[:, b, tsl]),
                         "affT": _c(affT[:, b, tsl]), "pT": _c(inp["p"][l, b, tsl].T),
                         "ple_norm": _c(inp["ple_norm"][l]), "ple_proj": _c(inp["ple_proj"][l]),
                         "ple_gate": _c(inp["ple_gate"][l]), "slotid": slotid,
                         "final_norm": _c(inp["final_norm"])})
        resE = run_spmd(ncE, maps)
        xT_c = [r["x3T"] for r in resE]
    out = np.concatenate([t.T for t in xT_c], axis=0).reshape(B_, T_, D_)
    return np.ascontiguousarray(out.astype(np.float32))
```
